# Optimizing a Trainium2 kernel written in Bass

```python
import math
import jax
import jax.numpy as jnp
from jax import lax
import numpy as np

D_MODEL = 1024
BATCH = 4
SEQ = 4096
DEPTH = 4

GRID_W = 64
CTX_LEN = 256

GLA_HEADS = 4
GLA_DK = 32
GLA_DV = 64
GLA_RANK = 16
GLA_TAU = 16.0
GLA_CHUNK = 32
ROPE_BASE = 10000.0
NA_HEADS = 4
NA_HD = 64
NA_WIN_R = 8
NA_WIN_C = 16
GDN_HEADS = 4
GDN_DK = 128
GDN_DV = 128
GDN_CONV = 5
GDN_CHUNK = 64
N_EXPERTS = 32
TOP_K = 4
D_EXPERT = D_MODEL
SWIGLU_LIMIT = 7.0
SWIGLU_ALPHA = 1.702
MOE_BLOCK = 256

N_MOD = 6
LN_EPS = 1e-5
RMS_EPS = 1e-6
DN_ALPHA = (2 * DEPTH) ** 0.25
DN_BETA = (8 * DEPTH) ** -0.25

IN_SPLITS = (GLA_HEADS * GLA_DK, GLA_HEADS * GLA_DK, GLA_HEADS * GLA_DV, 2 * GLA_RANK, GLA_HEADS * GLA_DV,
             NA_HEADS * NA_HD, NA_HEADS * NA_HD, NA_HEADS * NA_HD,
             GDN_HEADS * GDN_DK, GDN_HEADS * GDN_DK, GDN_HEADS * GDN_DV, 2 * GDN_HEADS, 2 * GDN_HEADS, GDN_HEADS * GDN_DV)
D_IN = sum(IN_SPLITS)
MIX_W = GLA_HEADS * GLA_DV + NA_HEADS * NA_HD + GDN_HEADS * GDN_DV

kernel_name = 'hybrid_gla_natten_gdn_moe_trunk'

F32 = jnp.float32


def _ln(t):
    tf = t.astype(F32)
    mu = tf.mean(-1, keepdims=True)
    var = jnp.mean(jnp.square(tf - mu), -1, keepdims=True)
    return ((tf - mu) * lax.rsqrt(var + LN_EPS)).astype(t.dtype)


def _post_ln(t, g, b):
    return _ln(t) * g + b


def _modulate(t, shift, scale):
    return _ln(t) * (1 + scale) + shift


def _rmsnorm(t, g):
    tf = t.astype(F32)
    y = tf * lax.rsqrt(jnp.mean(tf * tf, -1, keepdims=True) + RMS_EPS)
    return (y * g).astype(t.dtype)


def _l2norm(t):
    tf = t.astype(F32)
    return (tf * lax.rsqrt(jnp.sum(tf * tf, -1, keepdims=True) + 1e-6)).astype(t.dtype)


def _split_cols(p):
    return jnp.split(p, np.cumsum(IN_SPLITS)[:-1].tolist(), axis=-1)


def _axial_rope_tables(n_tok):
    t = jnp.arange(n_tok)
    half = GLA_DK // 2
    inv_freq = ROPE_BASE ** (-jnp.arange(0, half, 2, dtype=F32) / half)

    def cs(pos):
        ang = pos.astype(F32)[:, None] * inv_freq
        return jnp.cos(ang)[:, None, :], jnp.sin(ang)[:, None, :]

    return cs(t // GRID_W) + cs(t % GRID_W)


def _rotate(t, cos, sin):
    t1, t2 = jnp.split(t, 2, -1)
    return jnp.concatenate([t1 * cos - t2 * sin, t2 * cos + t1 * sin], -1)


def _axial_rope(t, rot):
    cos_r, sin_r, cos_c, sin_c = rot
    tr, tc = jnp.split(t, 2, -1)
    return jnp.concatenate([_rotate(tr, cos_r, sin_r), _rotate(tc, cos_c, sin_c)], -1).astype(t.dtype)


def _blocks(t, chunk):
    b_, T, H = t.shape[:3]
    return t.astype(F32).reshape(b_, T // chunk, chunk, H, -1).transpose(1, 0, 3, 2, 4)


def _unblocks(o, dtype):
    n, b_, H, C, d = o.shape
    return o.transpose(1, 0, 3, 2, 4).reshape(b_, n * C, H, d).astype(dtype)


def _gla_chunked(q, k, v, log_a, s0):
    C = GLA_CHUNK
    qc, kc, vc = _blocks(q, C), _blocks(k, C), _blocks(v, C)
    gc = jnp.cumsum(_blocks(log_a, C), axis=3)
    tril = jnp.tril(jnp.ones((C, C), dtype=bool))
    diff = jnp.where(tril[:, :, None], gc[..., :, None, :] - gc[..., None, :, :], -jnp.inf)
    attn = jnp.einsum('nbhtd,nbhsd,nbhtsd->nbhts', qc, kc, jnp.exp(diff))
    o_intra = jnp.einsum('nbhts,nbhsv->nbhtv', attn, vc)
    q_dec = qc * jnp.exp(gc)
    k_dec = kc * jnp.exp(gc[..., -1:, :] - gc)
    a_last = jnp.exp(gc[..., -1, :])

    def step(s, inp):
        qd, kd, vv, al = inp
        o = jnp.einsum('bhtk,bhkv->bhtv', qd, s)
        s = al[..., None] * s + jnp.einsum('bhtk,bhtv->bhkv', kd, vv)
        return s, o

    s_T, o_inter = lax.scan(step, s0.astype(F32), (q_dec, k_dec, vc, a_last))
    return _unblocks(o_intra + o_inter, v.dtype), s_T


def _gdn_chunked(q, k, v, g, beta, s0):
    C = GDN_CHUNK
    dv = v.shape[-1]
    qc, kc, vc = _blocks(q, C), _blocks(k, C), _blocks(v, C)
    gc = jnp.cumsum(_blocks(g[..., None], C)[..., 0], axis=-1)
    bc = _blocks(beta[..., None], C)[..., 0]
    tril = jnp.tril(jnp.ones((C, C), dtype=bool))
    strict = jnp.tril(jnp.ones((C, C), dtype=bool), -1)
    decay = jnp.exp(jnp.where(tril, gc[..., :, None] - gc[..., None, :], -jnp.inf))
    kb = kc * bc[..., None]
    a_mat = jnp.where(strict, jnp.einsum('nbhtk,nbhsk->nbhts', kb, kc) * decay, 0.0) + jnp.eye(C, dtype=F32)
    rhs = jnp.concatenate([vc * bc[..., None], kb * jnp.exp(gc)[..., None]], axis=-1)
    sol = lax.linalg.triangular_solve(a_mat, rhs, left_side=True, lower=True)
    u, w = sol[..., :dv], sol[..., dv:]
    qk = jnp.where(tril, jnp.einsum('nbhtk,nbhsk->nbhts', qc, kc) * decay, 0.0)
    q_dec = qc * jnp.exp(gc)[..., None]
    k_dec = kc * jnp.exp(gc[..., -1:] - gc)[..., None]
    a_last = jnp.exp(gc[..., -1])

    def step(s, inp):
        qd, kd, uu, ww, qkk, al = inp
        v_new = uu - jnp.einsum('bhtk,bhkv->bhtv', ww, s)
        o = jnp.einsum('bhtk,bhkv->bhtv', qd, s) + jnp.einsum('bhts,bhsv->bhtv', qkk, v_new)
        s = al[..., None, None] * s + jnp.einsum('bhtk,bhtv->bhkv', kd, v_new)
        return s, o

    s_T, o = lax.scan(step, s0.astype(F32), (q_dec, k_dec, u, w, qk, a_last))
    return _unblocks(o, v.dtype), s_T


def _bidirectional(scan, ctx_fwd, ctx_bwd, lat_fwd, lat_bwd, s0):
    flip = lambda args: tuple(jnp.flip(a, 1) for a in args)
    o_cf, s_f = scan(*ctx_fwd, s0)
    o_cb, s_b = scan(*flip(ctx_bwd), s0)
    o_lf, _ = scan(*lat_fwd, s_f)
    o_lb, _ = scan(*flip(lat_bwd), s_b)
    return o_lf + jnp.flip(o_lb, 1), o_cf + jnp.flip(o_cb, 1)


def _gla_group(cols_l, cols_c, rope, w_a2, b_a, norm_g):
    def prep(cols, rot):
        q, k, v, a_lr, g = cols
        b_, T = q.shape[:2]
        q = q.reshape(b_, T, GLA_HEADS, GLA_DK) * GLA_DK ** -0.5
        k = k.reshape(b_, T, GLA_HEADS, GLA_DK)
        if rot is not None:
            q, k = _axial_rope(q, rot), _axial_rope(k, rot)
        v = v.reshape(b_, T, GLA_HEADS, GLA_DV)
        z = jnp.einsum('btir,irk->btik', a_lr.reshape(b_, T, 2, GLA_RANK), w_a2) + b_a
        log_a = (jax.nn.log_sigmoid(z.astype(F32)) / GLA_TAU).reshape(b_, T, 2, GLA_HEADS, GLA_DK)
        return (q, k, v, log_a[:, :, 0]), (q, k, v, log_a[:, :, 1]), g

    fl, bl, g_l = prep(cols_l, rope)
    fc, bc, g_c = prep(cols_c, None)
    s0 = jnp.zeros((g_l.shape[0], GLA_HEADS, GLA_DK, GLA_DV), F32)
    o_l, o_c = _bidirectional(_gla_chunked, fc, bc, fl, bl, s0)
    out = lambda o, g: _rmsnorm(o, norm_g).reshape(g.shape) * jax.nn.silu(g)
    return out(o_l, g_l), out(o_c, g_c)


def _neighbourhood_attn(q, k, v, k_ctx, v_ctx, rpb):
    b_, S, H, hd = q.shape
    rows = S // GRID_W
    kr = min(NA_WIN_R, rows)
    scale = hd ** -0.5
    grid = lambda t: t.reshape(b_, rows, GRID_W, H, hd)
    qg, kg, vg = grid(q), grid(k), grid(v)
    r = jnp.arange(rows)
    r0 = jnp.clip(r - kr // 2, 0, rows - kr)
    key_rows = r0[:, None] + jnp.arange(kr)
    kb = kg[:, key_rows]
    vb = vg[:, key_rows].reshape(b_, rows, kr * GRID_W, H, hd)
    s_loc = jnp.einsum('brqhd,brikhd->bhrqik', qg, kb).astype(F32) * scale
    cidx = jnp.arange(GRID_W)
    c0 = jnp.clip(cidx - NA_WIN_C // 2, 0, GRID_W - NA_WIN_C)
    col_ok = (cidx[None, :] >= c0[:, None]) & (cidx[None, :] < c0[:, None] + NA_WIN_C)
    dr = key_rows - r[:, None]
    dc = jnp.clip(cidx[None, :] - cidx[:, None] + NA_WIN_C - 1, 0, 2 * NA_WIN_C - 2)
    bias = rpb[:, (dr + NA_WIN_R - 1)[:, None, :, None], dc[None, :, None, :]].astype(F32)
    s_loc = jnp.where(col_ok[:, None, :], s_loc + bias, -jnp.inf).reshape(b_, H, rows, GRID_W, kr * GRID_W)
    s_ctx = jnp.einsum('brqhd,blhd->bhrql', qg, k_ctx).astype(F32) * scale
    p = jax.nn.softmax(jnp.concatenate([s_loc, s_ctx], -1), axis=-1)
    n_loc = kr * GRID_W
    o = (jnp.einsum('bhrqj,brjhd->brqhd', p[..., :n_loc], vb.astype(F32))
         + jnp.einsum('bhrql,blhd->brqhd', p[..., n_loc:], v_ctx.astype(F32)))
    return o.reshape(b_, S, H * hd).astype(q.dtype)


def _na_group(cols_l, cols_c, rpb):
    heads = lambda t: t.reshape(t.shape[0], t.shape[1], NA_HEADS, NA_HD)
    ql, kl, vl = (heads(t) for t in cols_l)
    qc, kc, vc = (heads(t) for t in cols_c)
    o_l = _neighbourhood_attn(ql, kl, vl, kc, vc, rpb)
    s = jnp.einsum('bqhd,bkhd->bhqk', qc, kc).astype(F32) * NA_HD ** -0.5
    o_c = jnp.einsum('bhqk,bkhd->bqhd', jax.nn.softmax(s, axis=-1), vc.astype(F32))
    return o_l, o_c.reshape(qc.shape[0], qc.shape[1], -1).astype(qc.dtype)


def _dwconv_centred(t, w):
    pad = w.shape[0] // 2
    return lax.conv_general_dilated(t, w[:, None, :].astype(t.dtype), window_strides=(1,), padding=[(pad, pad)],
                                    dimension_numbers=('NWC', 'WIO', 'NWC'), feature_group_count=t.shape[-1])


def _gdn_group(cols_l, cols_c, conv_w, a_log, dt_bias, norm_g):
    def prep(cols):
        q, k, v, b_lr, a_lr, z = cols
        b_, T = q.shape[:2]
        qkv = jax.nn.silu(_dwconv_centred(jnp.concatenate([q, k, v], -1), conv_w))
        q, k, v = jnp.split(qkv, [GDN_HEADS * GDN_DK, 2 * GDN_HEADS * GDN_DK], axis=-1)
        q = _l2norm(q.reshape(b_, T, GDN_HEADS, GDN_DK)) * GDN_DK ** -0.5
        k = _l2norm(k.reshape(b_, T, GDN_HEADS, GDN_DK))
        v = v.reshape(b_, T, GDN_HEADS, GDN_DV)
        beta = jax.nn.sigmoid(b_lr.astype(F32)).reshape(b_, T, 2, GDN_HEADS)
        g = -jnp.exp(a_log.astype(F32)) * jax.nn.softplus(a_lr.astype(F32).reshape(b_, T, 2, GDN_HEADS) + dt_bias)
        return (q, k, v, g[:, :, 0], beta[:, :, 0]), (q, k, v, g[:, :, 1], beta[:, :, 1]), z

    fl, bl, z_l = prep(cols_l)
    fc, bc, z_c = prep(cols_c)
    s0 = jnp.zeros((z_l.shape[0], GDN_HEADS, GDN_DK, GDN_DV), F32)
    o_l, o_c = _bidirectional(_gdn_chunked, fc, bc, fl, bl, s0)
    out = lambda o, z: _rmsnorm(o, norm_g).reshape(z.shape) * jax.nn.silu(z)
    return out(o_l, z_l), out(o_c, z_c)


def _mixer(h_lat, h_ctx, rope, w_in, gla_w_a2, gla_b_a, gla_norm, na_rpb, gdn_conv, gdn_a_log, gdn_dt_bias, gdn_norm, w_out):
    cols_l = _split_cols(h_lat @ w_in)
    cols_c = _split_cols(h_ctx @ w_in)
    gla_l, gla_c = _gla_group(cols_l[0:5], cols_c[0:5], rope, gla_w_a2, gla_b_a, gla_norm)
    na_l, na_c = _na_group(cols_l[5:8], cols_c[5:8], na_rpb)
    gdn_l, gdn_c = _gdn_group(cols_l[8:], cols_c[8:], gdn_conv, gdn_a_log, gdn_dt_bias, gdn_norm)
    out_l = jnp.concatenate([gla_l, na_l, gdn_l], -1) @ w_out
    out_c = jnp.concatenate([gla_c, na_c, gdn_c], -1) @ w_out
    return out_l, out_c


def _moe(h, w_router, b_router, w_gate_up, b_gate_up, w_down, b_down):
    n_tok, d = h.shape
    logits = (h @ w_router + b_router).astype(F32)
    top_logit, top_e = lax.top_k(logits, TOP_K)
    gate = jax.nn.softmax(top_logit, axis=-1)
    nk = n_tok * TOP_K
    flat_e = top_e.reshape(-1).astype(jnp.int32)
    flat_tok = jnp.arange(nk, dtype=jnp.int32) // TOP_K
    order = jnp.argsort(flat_e)
    sorted_e = flat_e[order]
    counts = jnp.bincount(flat_e, length=N_EXPERTS)
    padded = (counts + MOE_BLOCK - 1) // MOE_BLOCK * MOE_BLOCK
    pad_end = jnp.cumsum(padded)
    pad_start = pad_end - padded
    start = jnp.cumsum(counts) - counts
    dest = pad_start[sorted_e] + jnp.arange(nk, dtype=jnp.int32) - start[sorted_e]
    n_blocks = -(-(nk + N_EXPERTS * (MOE_BLOCK - 1)) // MOE_BLOCK)
    buf_tok = jnp.full((n_blocks * MOE_BLOCK,), n_tok, jnp.int32).at[dest].set(flat_tok[order])
    block_e = jnp.minimum(jnp.searchsorted(pad_end, jnp.arange(n_blocks) * MOE_BLOCK, side='right'), N_EXPERTS - 1)
    h_pad = jnp.concatenate([h, jnp.zeros((1, d), h.dtype)], 0)
    xb = h_pad[buf_tok].reshape(n_blocks, MOE_BLOCK, d)

    def expert_block(args):
        xblk, e = args
        gu = xblk @ w_gate_up[e] + b_gate_up[e]
        gt, up = jnp.split(gu, 2, axis=-1)
        gt = jnp.minimum(gt, SWIGLU_LIMIT)
        up = jnp.clip(up, -SWIGLU_LIMIT, SWIGLU_LIMIT)
        act = (up + 1) * gt * jax.nn.sigmoid(SWIGLU_ALPHA * gt)
        return act @ w_down[e] + b_down[e]

    yb = lax.map(expert_block, (xb, block_e)).reshape(n_blocks * MOE_BLOCK, d)
    slot_pos = jnp.zeros((nk,), jnp.int32).at[order].set(dest)
    y = yb[slot_pos].reshape(n_tok, TOP_K, d)
    return jnp.einsum('nkd,nk->nd', y, gate.astype(y.dtype))


def setup_inputs(seed: int = 0) -> dict:
    key = jax.random.key(seed)
    ks = jax.random.split(key, 26)
    it = iter([ks[i] for i in range(26)])
    nrm = lambda shape, std: jax.random.normal(next(it), shape, F32) * std
    L, D, E, F = DEPTH, D_MODEL, N_EXPERTS, D_EXPERT
    x = nrm((BATCH, SEQ, D), 1.0)
    c = nrm((BATCH, D), 1.0)
    ctx = nrm((BATCH, CTX_LEN, D), 1.0)
    c_ctx = nrm((D,), 1.0)
    w_ada = nrm((L, D, N_MOD * D), 0.5 * D ** -0.5)
    b_ada = nrm((L, N_MOD * D), 0.02)
    w_in = nrm((L, D, D_IN), D ** -0.5)
    gla_w_a2 = nrm((L, 2, GLA_RANK, GLA_HEADS * GLA_DK), GLA_RANK ** -0.5)
    gla_b_a = nrm((L, 2, GLA_HEADS * GLA_DK), 0.1)
    gla_norm = 1.0 + nrm((L, GLA_DV), 0.02)
    na_rpb = nrm((L, NA_HEADS, 2 * NA_WIN_R - 1, 2 * NA_WIN_C - 1), 0.02)
    gdn_conv = nrm((L, GDN_CONV, GDN_HEADS * (2 * GDN_DK + GDN_DV)), GDN_CONV ** -0.5)
    gdn_a_log = jnp.log(jax.random.uniform(next(it), (L, 2, GDN_HEADS), F32, 1.0, 16.0))
    dt = jnp.exp(jax.random.uniform(next(it), (L, 2, GDN_HEADS), F32, math.log(1e-3), math.log(1e-1)))
    gdn_dt_bias = dt + jnp.log(-jnp.expm1(-dt))
    gdn_norm = 1.0 + nrm((L, GDN_DV), 0.02)
    w_out = nrm((L, MIX_W, D), DN_BETA * MIX_W ** -0.5)
    ln1_g = 1.0 + nrm((L, D), 0.02)
    ln1_b = nrm((L, D), 0.02)
    w_router = nrm((L, D, E), D ** -0.5)
    b_router = nrm((L, E), 0.01)
    w_gate_up = nrm((L, E, D, 2 * F), D ** -0.5)
    b_gate_up = nrm((L, E, 2 * F), 0.02)
    w_down = nrm((L, E, F, D), DN_BETA * F ** -0.5)
    b_down = nrm((L, E, D), 0.02)
    ln2_g = 1.0 + nrm((L, D), 0.02)
    ln2_b = nrm((L, D), 0.02)
    return {'x': x, 'c': c, 'ctx': ctx, 'c_ctx': c_ctx, 'w_ada': w_ada, 'b_ada': b_ada, 'w_in': w_in,
            'gla_w_a2': gla_w_a2, 'gla_b_a': gla_b_a, 'gla_norm': gla_norm, 'na_rpb': na_rpb,
            'gdn_conv': gdn_conv, 'gdn_a_log': gdn_a_log, 'gdn_dt_bias': gdn_dt_bias, 'gdn_norm': gdn_norm,
            'w_out': w_out, 'ln1_g': ln1_g, 'ln1_b': ln1_b, 'w_router': w_router, 'b_router': b_router,
            'w_gate_up': w_gate_up, 'b_gate_up': b_gate_up, 'w_down': w_down, 'b_down': b_down,
            'ln2_g': ln2_g, 'ln2_b': ln2_b}


def reference(x, c, ctx, c_ctx, w_ada, b_ada, w_in, gla_w_a2, gla_b_a, gla_norm, na_rpb,
              gdn_conv, gdn_a_log, gdn_dt_bias, gdn_norm, w_out, ln1_g, ln1_b,
              w_router, b_router, w_gate_up, b_gate_up, w_down, b_down, ln2_g, ln2_b):
    b_, S, D = x.shape
    rope = _axial_rope_tables(S)
    sc = jax.nn.silu(c)
    scc = jax.nn.silu(c_ctx)
    for l in range(DEPTH):
        m_lat = jnp.split((sc @ w_ada[l] + b_ada[l])[:, None, :], N_MOD, axis=-1)
        m_ctx = jnp.split(scc @ w_ada[l] + b_ada[l], N_MOD, axis=-1)
        a_lat, a_ctx = _mixer(_modulate(x, m_lat[0], m_lat[1]), _modulate(ctx, m_ctx[0], m_ctx[1]), rope,
                              w_in[l], gla_w_a2[l], gla_b_a[l], gla_norm[l], na_rpb[l], gdn_conv[l],
                              gdn_a_log[l], gdn_dt_bias[l], gdn_norm[l], w_out[l])
        x = _post_ln(DN_ALPHA * x + m_lat[2] * a_lat, ln1_g[l], ln1_b[l])
        moe_w = (w_router[l], b_router[l], w_gate_up[l], b_gate_up[l], w_down[l], b_down[l])
        h_lat = _modulate(x, m_lat[3], m_lat[4]).reshape(b_ * S, D)
        if l == DEPTH - 1:
            f_lat = _moe(h_lat, *moe_w)
        else:
            ctx = _post_ln(DN_ALPHA * ctx + m_ctx[2] * a_ctx, ln1_g[l], ln1_b[l])
            h_ctx = _modulate(ctx, m_ctx[3], m_ctx[4]).reshape(-1, D)
            f = _moe(jnp.concatenate([h_lat, h_ctx], 0), *moe_w)
            f_lat = f[:b_ * S]
            ctx = _post_ln(DN_ALPHA * ctx + m_ctx[5] * f[b_ * S:].reshape(ctx.shape), ln2_g[l], ln2_b[l])
        x = _post_ln(DN_ALPHA * x + m_lat[5] * f_lat.reshape(b_, S, D), ln2_g[l], ln2_b[l])
    return x
```

```python
import numpy as np
from contextlib import ExitStack
import concourse.bass as bass
import concourse.mybir as mybir
from concourse.bass_utils import run_bass_kernel_spmd

F32 = mybir.dt.float32
BF16 = mybir.dt.bfloat16
AF = mybir.ActivationFunctionType
ALU = mybir.AluOpType

ENGS = ["sync", "scalar", "gpsimd", "vector", "tensor"]
NDSEM = 6

D = 1024
DEPTH = 4
NB = 4
SEQ = 4096
CTX = 256
NE = 32
ALPHA = (2 * DEPTH) ** 0.25
LN_EPS = 1e-5


class Prog:
    def __init__(self):
        self.nc = bass.Bass("TRN2", target_bir_lowering=False)
        self.ops = []
        self.glob = ExitStack()
        self.stage = ExitStack()
        self.sems = {}
        self.cnt = {e: 0 for e in ENGS}
        self.dcnt = {}
        self.dnext = {e: 0 for e in ENGS}
        self.drams = {}
        self.nstage = 0

    def dram(self, name, shape, dt=F32, kind="ExternalInput"):
        if name in self.drams:
            return self.drams[name]
        t = self.nc.dram_tensor(name, list(shape), dt, kind=kind)
        self.drams[name] = t
        return t

    def sb(self, name, shape, dt=F32):
        return self.stage.enter_context(self.nc.sbuf_tensor(f"g{self.nstage}_{name}", list(shape), dt))

    def ps(self, name, shape, dt=F32):
        return self.stage.enter_context(self.nc.psum_tensor(f"g{self.nstage}_{name}", list(shape), dt))

    def op(self, eng, fn, r=(), w=(), dma=False):
        self.ops.append((eng, fn, tuple(r), tuple(w), dma))

    def dma(self, out, in_, r=(), w=(), q="sync", **kw):
        self.op(q, lambda e: e.dma_start(out=out, in_=in_, **kw), r, w, dma=True)

    def V(self, fn, r=(), w=()):
        self.op("vector", fn, r, w)

    def A(self, fn, r=(), w=()):
        self.op("scalar", fn, r, w)

    def G(self, fn, r=(), w=()):
        self.op("gpsimd", fn, r, w)

    def T(self, fn, r=(), w=()):
        self.op("tensor", fn, r, w)

    def _sem(self, key):
        if key not in self.sems:
            self.sems[key] = self.glob.enter_context(
                self.nc.semaphore("s_" + "_".join(str(x) for x in key) + f"_{len(self.sems)}"))
        return self.sems[key]

    def end_stage(self):
        nc = self.nc
        ops = self.ops
        self.ops = []
        n = len(ops)
        if n:
            start_final = {k: v for k, v in self._final().items()}
            tok = [None] * n
            pre_wait = [[] for _ in range(n)]
            for i, (eng, fn, r, w, dma) in enumerate(ops):
                if dma:
                    slot = self.dnext[eng] % NDSEM
                    self.dnext[eng] += 1
                    key = ("d", eng, slot)
                    prev = self.dcnt.get(key, 0)
                    if prev > 0:
                        pre_wait[i].append((key, prev * 16))
                    self.dcnt[key] = prev + 1
                    tok[i] = (key, (prev + 1) * 16)
                else:
                    self.cnt[eng] += 1
                    tok[i] = (("c", eng), self.cnt[eng])
            last_w = {}
            readers = {}
            waits = [None] * n
            for i, (eng, fn, r, w, dma) in enumerate(ops):
                deps = set()
                for res in r:
                    if res in last_w:
                        deps.add(last_w[res])
                for res in w:
                    if res in last_w:
                        deps.add(last_w[res])
                    for j in readers.get(res, ()):
                        deps.add(j)
                deps.discard(i)
                waits[i] = [tok[j] for j in deps] + pre_wait[i]
                for res in r:
                    readers.setdefault(res, []).append(i)
                for res in w:
                    last_w[res] = i
                    readers[res] = []
            for t in tok:
                self._sem(t[0])
            final = self._final()
            per_eng = {e: [] for e in ENGS}
            for i in range(n):
                per_eng[ops[i][0]].append(i)
            sems = self.sems

            def body(ename):
                def run(eng):
                    known = dict(start_final)
                    for i in per_eng[ename]:
                        _, fn, r, w, dma = ops[i]
                        need = {}
                        for (k, v) in waits[i]:
                            if v > need.get(k, 0):
                                need[k] = v
                        for k, v in need.items():
                            if known.get(k, 0) >= v:
                                continue
                            eng.wait_ge(sems[k], v)
                            known[k] = v
                        ins = fn(eng)
                        k, v = tok[i]
                        ins.then_inc(sems[k], 16 if dma else 1)
                    for k, v in final.items():
                        if known.get(k, 0) < v:
                            eng.wait_ge(sems[k], v)
                return run

            with nc.Block() as blk:
                blk.sync(body("sync"))
                blk.scalar(body("scalar"))
                blk.gpsimd(body("gpsimd"))
                blk.vector(body("vector"))
                blk.tensor(body("tensor"))
        self.stage.close()
        self.stage = ExitStack()
        self.nstage += 1

    def _final(self):
        f = {("c", e): c for e, c in self.cnt.items() if c > 0}
        for k, c in self.dcnt.items():
            f[k] = c * 16
        return f

    def finish(self):
        self.end_stage()
        self.glob.close()
        return self.nc


def build_stage0():
    p = Prog()
    cc = p.dram("cc", [5, D])
    wa = p.dram("wa", [DEPTH, D, 768])
    ba = p.dram("ba", [DEPTH, 768])
    ident_d = p.dram("ident", [128, 128])
    out = p.dram("m", [DEPTH, 5, 768], kind="ExternalOutput")
    ident = p.sb("ident_s", [128, 128])
    cs = p.sb("cs", [5, D])
    ss = p.sb("ss", [5, D])
    scT = p.sb("scT", [128, 8, 5])
    wb = [p.sb(f"w{i}", [128, 8, 768]) for i in range(2)]
    bb = [p.sb(f"bb{i}", [5, 768]) for i in range(2)]
    ob = [p.sb(f"ob{i}", [5, 768]) for i in range(2)]
    pt = p.ps("pt", [128, 512])
    pm = [p.ps(f"pm{i}", [128, 512]) for i in range(2)]
    p.dma(ident[:], ident_d.ap(), w=["ident"])
    p.dma(cs[:], cc.ap(), w=["cs"])
    p.A(lambda e: e.activation(out=ss[:], in_=cs[:], func=AF.Silu), r=["cs"], w=["ss"])
    def tr0(e):
        ins = None
        for c in range(8):
            ins = e.transpose(pt[:, c * 8:c * 8 + 5], ss[0:5, c * 128:(c + 1) * 128], ident[0:5, 0:5])
        return ins
    p.T(tr0, r=["ss", "ident"], w=["pt"])
    p.V(lambda e: e.tensor_copy(out=scT[:, :, :], in_=pt[:, 0:64].rearrange("p (c k) -> p c k", k=8)[:, :, 0:5]),
        w=["pt", "scT"])
    for l in range(DEPTH):
        b = l % 2
        src = wa.ap()[l].rearrange("(c p) f -> p c f", p=128)
        for c in range(8):
            p.dma(wb[b][:, c, :], src[:, c, :], w=[f"w{b}"], q="sync" if c % 2 == 0 else "gpsimd")
        p.dma(bb[b][:], ba.ap()[l:l + 1, :].partition_broadcast(5), w=[f"bb{b}"])
        for h in range(2):
            def mm(e, b=b, h=h):
                ins = None
                for c in range(8):
                    ins = e.matmul(pm[h][0:5, 0:384], lhsT=scT[:, c, :], rhs=wb[b][:, c, h * 384:(h + 1) * 384],
                                   start=(c == 0), stop=(c == 7))
                return ins
            p.T(mm, r=["scT", f"w{b}"], w=[f"pm{h}"])
            p.V(lambda e, b=b, h=h: e.tensor_tensor(out=ob[b][:, h * 384:(h + 1) * 384], in0=pm[h][0:5, 0:384],
                                                    in1=bb[b][:, h * 384:(h + 1) * 384], op=ALU.add),
                r=[f"bb{b}"], w=[f"pm{h}", f"ob{b}"])
        p.dma(out.ap()[l], ob[b][:], r=[f"ob{b}"])
    return p.finish()


NTB = 17
TB = NTB * 128


def build_stageB(n_units=64, do_b3=True):
    p = Prog()
    xin = p.dram("xin", [TB, D])
    pa = p.dram("pa", [TB, D])
    pbd = p.dram("pb", [TB, D])
    vecs = p.dram("vecs", [128, 128])
    ident_d = p.dram("ident", [128, 128])
    wr = p.dram("wr", [D, NE])
    br = p.dram("br", [1, NE])
    wgu = p.dram("wgu", [NE, D, 2 * D])
    bgu = p.dram("bgu", [NE * 16, 128])
    wdn = p.dram("wdn", [NE, D, D])
    bdn = p.dram("bdn", [NE, D])
    x1s = p.dram("x1s", [TB, D], kind="Internal")
    xout = p.dram("xout", [TB, D], kind="ExternalOutput")

    ident = p.sb("ident_s", [128, 128])
    vs = p.sb("vs", [128, 128])
    vT = p.sb("vT", [128, 128])
    onep = p.sb("onep", [128, 16])
    bgs = p.sb("bgs", [128, 4, 128])
    bguT = p.sb("bguT", [128, 512])
    wr_s = p.sb("wr_s", [128, 8, NE])
    br_s = p.sb("br_s", [128, NE])
    hT = p.sb("hT", [128, 8, TB], BF16)
    hT32 = p.sb("hT32", [128, 8, 128])
    acc = p.sb("acc", [128, NTB, D])
    gw = p.sb("gw", [128, NTB, NE])
    bc = [p.sb(f"bc{i}", [128, D]) for i in range(4)]
    R = [p.sb(f"R{i}", [128, D]) for i in range(4)]
    st = p.sb("st", [128, 16])
    rt = p.sb("rt", [128, 4, NE])
    m8 = p.sb("m8", [128, 8])
    ones_b = p.sb("ones_b", [1, 128], BF16)
    bd_b = [p.sb(f"bd_b{i}", [1, D], BF16) for i in range(2)]
    wg_s = [p.sb(f"wg{i}", [128, 8, 512], BF16) for i in range(2)]
    wu_s = [p.sb(f"wu{i}", [128, 8, 512], BF16) for i in range(2)]
    wd_s = [p.sb(f"wd{i}", [128, 4, D], BF16) for i in range(2)]
    actT = [p.sb(f"actT{i}", [128, 4, 512], BF16) for i in range(2)]
    psA = p.ps("psA", [128, 1024])
    psB = [p.ps(f"psB{i}", [128, 512]) for i in range(6)]

    vrow = vecs.ap().rearrange("(v c) p -> v (c p)", c=8)

    def bcast(dst, v, name):
        p.dma(dst[:], vrow[v:v + 1, :].partition_broadcast(128), w=[name])

    p.dma(ident[:], ident_d.ap(), w=["ident"])
    p.dma(vs[:], vecs.ap(), w=["vs"])
    p.dma(wr_s[:], wr.ap().rearrange("(c p) e -> p c e", p=128), w=["wr_s"])
    p.dma(br_s[:], br.ap().partition_broadcast(128), w=["br_s"])
    p.dma(bgs[:], bgu.ap().rearrange("(a p) f -> p a f", p=128), w=["bgs"])
    p.V(lambda e: e.memset(ones_b[:], 1.0), w=["ones_b"])
    p.G(lambda e: e.memset(acc[:], 0.0), w=[("acc", i) for i in range(NTB)])
    p.T(lambda e: e.transpose(psB[0][:, 0:128], vs[:], ident[:]), r=["vs", "ident"], w=["psB0"])
    p.V(lambda e: e.tensor_copy(out=vT[:], in_=psB[0][:, 0:128]), w=["psB0", "vT"])
    p.V(lambda e: e.tensor_scalar(out=onep[:, 0:8], in0=vT[:, 32:40], scalar1=1.0, scalar2=None, op0=ALU.add),
        r=["vT"], w=["onep"])
    p.V(lambda e: e.tensor_scalar(out=onep[:, 8:16], in0=vT[:, 80:88], scalar1=1.0, scalar2=None, op0=ALU.add),
        r=["vT", "onep"], w=["onep"])
    def trb(e):
        ins = None
        for a in range(4):
            ins = e.transpose(psB[1][:, a * 128:(a + 1) * 128], bgs[:, a, :], ident[:])
        return ins
    p.T(trb, r=["bgs", "ident"], w=["psB1"])
    p.V(lambda e: e.tensor_copy(out=bguT[:], in_=psB[1][:, :]), w=["psB1", "bguT"])

    def ln_stats(src, srcname, tmp, tmpname, junk, junkname, col):
        p.V(lambda e: e.reduce_sum(out=st[:, col + 2:col + 3], in_=src[:], axis=mybir.AxisListType.X),
            r=[srcname], w=["st"])
        p.V(lambda e: e.tensor_scalar(out=st[:, col:col + 1], in0=st[:, col + 2:col + 3], scalar1=1.0 / D,
                                      scalar2=None, op0=ALU.mult), r=["st"], w=["st"])
        p.V(lambda e: e.tensor_scalar(out=tmp[:], in0=src[:], scalar1=st[:, col:col + 1], scalar2=None,
                                      op0=ALU.subtract), r=[srcname, "st"], w=[tmpname])
        p.A(lambda e: e.activation(out=junk[:], in_=tmp[:], func=AF.Square, accum_out=st[:, col + 3:col + 4]),
            r=[tmpname], w=[junkname, "st"])
        p.V(lambda e: e.tensor_scalar(out=st[:, col + 1:col + 2], in0=st[:, col + 3:col + 4], scalar1=1.0 / D,
                                      scalar2=LN_EPS, op0=ALU.mult, op1=ALU.add), r=["st"], w=["st"])
        p.A(lambda e: e.activation(out=st[:, col + 1:col + 2], in_=st[:, col + 1:col + 2], func=AF.Sqrt),
            r=["st"], w=["st"])
        p.V(lambda e: e.reciprocal(out=st[:, col + 1:col + 2], in_=st[:, col + 1:col + 2]), r=["st"], w=["st"])

    bcast(bc[0], 2, "bc0")
    bcast(bc[1], 8, "bc1")
    bcast(bc[2], 12, "bc2")
    bcast(bc[3], 13, "bc3")
    for i in range(NTB):
        isctx = (i == NTB - 1)
        rows = slice(i * 128, (i + 1) * 128)
        p.dma(R[0][:], xin.ap()[rows, :], w=["R0"])
        p.dma(R[1][:], pa.ap()[rows, :], w=["R1"], q="gpsimd")
        p.dma(R[2][:], pbd.ap()[rows, :], w=["R2"])
        g1 = bc[1] if isctx else bc[0]
        g1n = "bc1" if isctx else "bc0"
        p.G(lambda e: e.tensor_tensor(out=R[1][:], in0=R[1][:], in1=R[2][:], op=ALU.add), r=["R1", "R2"], w=["R1"])
        p.V(lambda e, g1=g1: e.tensor_tensor(out=R[1][:], in0=R[1][:], in1=g1[:], op=ALU.mult), r=["R1", g1n], w=["R1"])
        p.V(lambda e: e.scalar_tensor_tensor(out=R[0][:], in0=R[0][:], scalar=ALPHA, in1=R[1][:], op0=ALU.mult,
                                             op1=ALU.add), r=["R0", "R1"], w=["R0"])
        ln_stats(R[0], "R0", R[2], "R2", R[1], "R1", 0)
        p.V(lambda e: e.scalar_tensor_tensor(out=R[1][:], in0=R[2][:], scalar=st[:, 1:2], in1=bc[2][:], op0=ALU.mult,
                                             op1=ALU.mult), r=["R2", "st", "bc2"], w=["R1"])
        p.G(lambda e: e.tensor_tensor(out=R[1][:], in0=R[1][:], in1=bc[3][:], op=ALU.add), r=["R1", "bc3"], w=["R1"])
        p.dma(x1s.ap()[rows, :], R[1][:], r=["R1"], w=[("x1s", i)])
        ln_stats(R[1], "R1", R[0], "R0", R[2], "R2", 4)
        p.V(lambda e: e.tensor_scalar(out=R[0][:], in0=R[0][:], scalar1=st[:, 5:6], scalar2=None, op0=ALU.mult),
            r=["R0", "st"], w=["R0"])
        for bk in range(2):
            def trh(e, bk=bk):
                ins = None
                for c in range(bk * 4, bk * 4 + 4):
                    ins = e.transpose(psA[:, c * 128:(c + 1) * 128], R[0][:, c * 128:(c + 1) * 128], ident[:])
                return ins
            p.T(trh, r=["R0", "ident"], w=[f"psA{bk}"])
        sc_off = 8 if isctx else 0
        sh_off = 72 if isctx else 24
        for c in range(8):
            p.V(lambda e, c=c, so=sc_off, sh=sh_off: e.tensor_scalar(
                out=hT32[:, c, :], in0=psA[:, c * 128:(c + 1) * 128], scalar1=onep[:, so + c:so + c + 1],
                scalar2=vT[:, sh + c:sh + c + 1], op0=ALU.mult, op1=ALU.add),
                r=["onep", "vT"], w=[f"psA{c // 4}", ("hT32", c)])
            p.A(lambda e, c=c, i=i: e.copy(out=hT[:, c, i * 128:(i + 1) * 128], in_=hT32[:, c, :]),
                r=[("hT32", c)], w=[("hT", i)])

        def router(e):
            ins = None
            for c in range(8):
                ins = e.matmul(psB[2][:, 0:NE], lhsT=hT32[:, c, :], rhs=wr_s[:, c, :], start=(c == 0), stop=(c == 7))
            return ins
        p.T(router, r=[("hT32", c) for c in range(8)] + ["wr_s"], w=["psB2"])
        L, EX, SEL = rt[:, 0, :], rt[:, 1, :], rt[:, 2, :]
        p.V(lambda e: e.tensor_tensor(out=L, in0=psB[2][:, 0:NE], in1=br_s[:], op=ALU.add), r=["br_s"], w=["psB2", "rt"])
        p.V(lambda e: e.max(out=m8[:], in_=L), r=["rt"], w=["m8"])
        p.V(lambda e: e.tensor_scalar(out=st[:, 8:9], in0=m8[:, 0:1], scalar1=-1.0, scalar2=None, op0=ALU.mult),
            r=["m8"], w=["st"])
        p.A(lambda e: e.activation(out=EX, in_=L, func=AF.Exp, bias=st[:, 8:9], scale=1.0), r=["rt", "st"], w=["rt"])
        p.V(lambda e: e.tensor_scalar(out=SEL, in0=L, scalar1=m8[:, 3:4], scalar2=None, op0=ALU.is_ge),
            r=["rt", "m8"], w=["rt"])
        p.V(lambda e: e.tensor_tensor(out=EX, in0=EX, in1=SEL, op=ALU.mult), r=["rt"], w=["rt"])
        p.V(lambda e: e.reduce_sum(out=st[:, 9:10], in_=EX, axis=mybir.AxisListType.X), r=["rt"], w=["st"])
        p.V(lambda e: e.reciprocal(out=st[:, 10:11], in_=st[:, 9:10]), r=["st"], w=["st"])
        p.V(lambda e, i=i: e.tensor_scalar(out=gw[:, i, :], in0=EX, scalar1=st[:, 10:11], scalar2=None, op0=ALU.mult),
            r=["rt", "st"], w=[("gw", i)])

    dummy = st[:, 15:16]
    RN = ["R0", "R1", "R2", "R3", ("R2", "u"), ("R2", "t"), ("R3", "u"), ("R3", "t")]
    p.V(lambda e: e.memset(dummy, 0.0), r=RN, w=RN)
    for ex in range(NE):
        p.V(lambda e, ex=ex: e.tensor_scalar(out=bguT[:, ex * 16 + 8:ex * 16 + 16], in0=bguT[:, ex * 16 + 8:ex * 16 + 16],
                                             scalar1=1.0, scalar2=None, op0=ALU.add), r=["bguT"], w=["bguT"])
    groups = [(0, 4), (4, 4), (8, 4), (12, 4), (16, 1)]
    units = [(ex, hf) for ex in range(NE) for hf in range(2)][:n_units]

    def issue_loads(u):
        ex, hf = units[u]
        b = u % 2
        gsrc = wgu.ap()[ex].rearrange("(c p) f -> p c f", p=128)
        p.dma(wg_s[b][:], gsrc[:, :, hf * 512:(hf + 1) * 512], w=[f"wg{b}"], q="gpsimd")
        p.dma(wu_s[b][:], gsrc[:, :, D + hf * 512:D + (hf + 1) * 512], w=[f"wu{b}"], q="gpsimd")
        dsrc = wdn.ap()[ex, hf * 512:(hf + 1) * 512, :].rearrange("(j p) d -> p j d", p=128)
        p.dma(wd_s[b][:], dsrc, w=[f"wd{b}"], q="gpsimd")
        if hf == 0:
            eb = ex % 2
            p.dma(bd_b[eb][:], bdn.ap()[ex:ex + 1, :], w=[f"bd{eb}"], q="gpsimd")

    if units:
        issue_loads(0)
    actc = 0
    for u, (ex, hf) in enumerate(units):
        b = u % 2
        if u + 1 < len(units):
            issue_loads(u + 1)
        for gi, (t0, nt) in enumerate(groups):
            N = nt * 128
            ab = actc % 2
            actc += 1
            A_ = actT[ab]
            An = f"actT{ab}"
            for j in range(4):
                fi = ex * 16 + hf * 4 + j
                ui = ex * 16 + 8 + hf * 4 + j
                s = j % 2
                pg = psB[s]
                pu = psB[2 + s]
                pgn, pun = f"psB{s}", f"psB{2 + s}"

                def mmg(e, b=b, j=j, pg=pg, t0=t0, N=N):
                    ins = None
                    for c in range(8):
                        ins = e.matmul(pg[:, 0:N], lhsT=wg_s[b][:, c, j * 128:(j + 1) * 128],
                                       rhs=hT[:, c, t0 * 128:t0 * 128 + N], start=(c == 0), stop=(c == 7))
                    return ins

                def mmu(e, b=b, j=j, pu=pu, t0=t0, N=N):
                    ins = None
                    for c in range(8):
                        ins = e.matmul(pu[:, 0:N], lhsT=wu_s[b][:, c, j * 128:(j + 1) * 128],
                                       rhs=hT[:, c, t0 * 128:t0 * 128 + N], start=(c == 0), stop=(c == 7))
                    return ins
                hr = [("hT", t0 + k) for k in range(nt)]
                p.T(mmg, r=[f"wg{b}"] + hr, w=[pgn])
                p.T(mmu, r=[f"wu{b}"] + hr, w=[pun])
                gt = R[0 + s][:, 0:512]
                sg = R[0 + s][:, 512:1024]
                u1 = R[2 + s][:, 0:512]
                t1 = R[2 + s][:, 512:1024]
                gn, un, tn = f"R{s}", (f"R{2 + s}", "u"), (f"R{2 + s}", "t")
                p.V(lambda e, gt=gt, pg=pg, fi=fi, N=N: e.tensor_scalar(
                    out=gt[:, 0:N], in0=pg[:, 0:N], scalar1=bguT[:, fi:fi + 1], scalar2=7.0, op0=ALU.add,
                    op1=ALU.min), r=["bguT"], w=[pgn, gn])
                p.A(lambda e, gt=gt, sg=sg, N=N: e.activation(out=sg[:, 0:N], in_=gt[:, 0:N], func=AF.Sigmoid,
                                                              scale=1.702), r=[gn], w=[gn])
                p.V(lambda e, u1=u1, pu=pu, ui=ui, N=N: e.tensor_scalar(
                    out=u1[:, 0:N], in0=pu[:, 0:N], scalar1=bguT[:, ui:ui + 1], scalar2=8.0, op0=ALU.add,
                    op1=ALU.min), r=["bguT"], w=[pun, un])
                p.G(lambda e, gt=gt, sg=sg, t1=t1, N=N: e.tensor_tensor(out=t1[:, 0:N], in0=gt[:, 0:N],
                                                                       in1=sg[:, 0:N], op=ALU.mult),
                    r=[gn], w=[tn])
                p.V(lambda e, t1=t1, u1=u1, A_=A_, j=j, N=N: e.scalar_tensor_tensor(
                    out=A_[:, j, 0:N], in0=u1[:, 0:N], scalar=-6.0, in1=t1[:, 0:N], op0=ALU.max, op1=ALU.mult),
                    r=[tn, un], w=[(An, j)])
            for k in range(nt):
                ti = t0 + k
                for dh in range(2):
                    yb = 4 + (2 * k + dh) % 2
                    py = psB[yb]

                    def mmy(e, b=b, k=k, dh=dh, py=py, A_=A_, hf=hf, ex=ex):
                        ins = None
                        for j in range(4):
                            ins = e.matmul(py[:, :], lhsT=A_[:, j, k * 128:(k + 1) * 128],
                                           rhs=wd_s[b][:, j, dh * 512:(dh + 1) * 512], start=(j == 0),
                                           stop=(j == 3 and hf == 1))
                        if hf == 0:
                            ins = e.matmul(py[:, :], lhsT=ones_b[0:1, :],
                                           rhs=bd_b[ex % 2][0:1, dh * 512:(dh + 1) * 512], start=False, stop=True)
                        return ins
                    rr = [(An, j) for j in range(4)] + [f"wd{b}"]
                    if hf == 0:
                        rr += ["ones_b", f"bd{ex % 2}"]
                    p.T(mmy, r=rr, w=[f"psB{yb}"])
                    p.V(lambda e, ti=ti, dh=dh, py=py, ex=ex: e.scalar_tensor_tensor(
                        out=acc[:, ti, dh * 512:(dh + 1) * 512], in0=py[:, :], scalar=gw[:, ti, ex:ex + 1],
                        in1=acc[:, ti, dh * 512:(dh + 1) * 512], op0=ALU.mult, op1=ALU.add),
                        r=[("gw", ti), ("acc", ti)], w=[f"psB{yb}", ("acc", ti)])

    p.V(lambda e: e.memset(dummy, 0.0), r=RN, w=RN)
    bcast(bc[0], 5, "bc0")
    bcast(bc[1], 11, "bc1")
    bcast(bc[2], 14, "bc2")
    bcast(bc[3], 15, "bc3")
    for i in range(NTB if do_b3 else 0):
        isctx = (i == NTB - 1)
        rows = slice(i * 128, (i + 1) * 128)
        X, Y, Z = R[0], R[1], R[2]
        Xn, Yn, Zn = "R0", "R1", "R2"
        p.dma(X[:], x1s.ap()[rows, :], r=[("x1s", i)], w=[Xn])
        g2 = bc[1] if isctx else bc[0]
        g2n = "bc1" if isctx else "bc0"
        p.V(lambda e, i=i, g2=g2, Y=Y: e.tensor_tensor(out=Y[:], in0=acc[:, i, :], in1=g2[:], op=ALU.mult),
            r=[("acc", i), g2n], w=[Yn])
        p.V(lambda e, X=X, Y=Y: e.scalar_tensor_tensor(out=X[:], in0=X[:], scalar=ALPHA, in1=Y[:], op0=ALU.mult,
                                                       op1=ALU.add), r=[Xn, Yn], w=[Xn])
        ln_stats(X, Xn, Z, Zn, Y, Yn, 0)
        p.V(lambda e, Y=Y, Z=Z: e.scalar_tensor_tensor(out=Y[:], in0=Z[:], scalar=st[:, 1:2], in1=bc[2][:],
                                                       op0=ALU.mult, op1=ALU.mult), r=[Zn, "st", "bc2"], w=[Yn])
        p.G(lambda e, Y=Y: e.tensor_tensor(out=Y[:], in0=Y[:], in1=bc[3][:], op=ALU.add), r=[Yn, "bc3"], w=[Yn])
        p.dma(xout.ap()[rows, :], Y[:], r=[Yn])
    return p.finish()


NTA = 34
TA = NTA * 128
FROWS = 1056
TCOLS = 776
F_GROUPS = [(0, 32), (32, 32), (64, 32), (96, 32), (128, 32), (160, 128), (288, 128), (416, 128), (544, 128),
            (672, 128), (800, 128), (928, 128)]
TOK_GROUPS = [(g * 512, 512) for g in range(8)] + [(4096, 256)]


def ln_stats_generic(p, st, src, srcname, tmp, tmpname, junk, junkname, col, stname="st"):
    X_ = mybir.AxisListType.X
    p.V(lambda e: e.reduce_sum(out=st[:, col + 2:col + 3], in_=src[:], axis=X_), r=[srcname], w=[stname])
    p.V(lambda e: e.tensor_scalar(out=st[:, col:col + 1], in0=st[:, col + 2:col + 3], scalar1=1.0 / D,
                                  scalar2=None, op0=ALU.mult), r=[stname], w=[stname])
    p.V(lambda e: e.tensor_scalar(out=tmp[:], in0=src[:], scalar1=st[:, col:col + 1], scalar2=None,
                                  op0=ALU.subtract), r=[srcname, stname], w=[tmpname])
    p.A(lambda e: e.activation(out=junk[:], in_=tmp[:], func=AF.Square, accum_out=st[:, col + 3:col + 4]),
        r=[tmpname], w=[junkname, stname])
    p.V(lambda e: e.tensor_scalar(out=st[:, col + 1:col + 2], in0=st[:, col + 3:col + 4], scalar1=1.0 / D,
                                  scalar2=LN_EPS, op0=ALU.mult, op1=ALU.add), r=[stname], w=[stname])
    p.A(lambda e: e.activation(out=st[:, col + 1:col + 2], in_=st[:, col + 1:col + 2], func=AF.Sqrt),
        r=[stname], w=[stname])
    p.V(lambda e: e.reciprocal(out=st[:, col + 1:col + 2], in_=st[:, col + 1:col + 2]), r=[stname], w=[stname])


def emit_A1(p):
    xc = p.dram("xc", [TA, D])
    vecs = p.dram("vecs", [128, 128])
    ident_d = p.dram("ident", [128, 128])
    wF = p.dram("wF", [D, FROWS])
    wT = p.dram("wT", [D, TCOLS])
    outF = p.dram("outF", [FROWS, TA], kind="ExternalOutput")
    outT = p.dram("outT", [TA, TCOLS], kind="ExternalOutput")
    ident = p.sb("ident_s", [128, 128])
    vs = p.sb("vs", [128, 128])
    vT = p.sb("vT", [128, 128])
    onep = p.sb("onep", [128, 16])
    hT = p.sb("hT", [128, 8, TA], BF16)
    wF_s = p.sb("wF_s", [128, 8, FROWS], BF16)
    wT_s = p.sb("wT_s", [128, 8, TCOLS], BF16)
    X = [p.sb(f"X{i}", [128, D]) for i in range(3)]
    st = p.sb("st", [128, 16])
    stg = [p.sb(f"stg{i}", [128, 512]) for i in range(4)]
    psA = p.ps("psA", [128, 1024])
    psB = [p.ps(f"psB{i}", [128, 512]) for i in range(4)]
    p.dma(ident[:], ident_d.ap(), w=["ident"])
    p.dma(vs[:], vecs.ap(), w=["vs"])
    p.dma(wF_s[:], wF.ap().rearrange("(c p) f -> p c f", p=128), w=["wF_s"], q="gpsimd")
    p.dma(wT_s[:], wT.ap().rearrange("(c p) f -> p c f", p=128), w=["wT_s"], q="gpsimd")
    p.T(lambda e: e.transpose(psB[0][:, 0:128], vs[:], ident[:]), r=["vs", "ident"], w=["psB0"])
    p.V(lambda e: e.tensor_copy(out=vT[:], in_=psB[0][:, 0:128]), w=["psB0", "vT"])
    p.V(lambda e: e.tensor_scalar(out=onep[:, 0:8], in0=vT[:, 8:16], scalar1=1.0, scalar2=None, op0=ALU.add),
        r=["vT"], w=["onep"])
    p.V(lambda e: e.tensor_scalar(out=onep[:, 8:16], in0=vT[:, 56:64], scalar1=1.0, scalar2=None, op0=ALU.add),
        r=["vT"], w=["onep"])
    for i in range(NTA):
        isctx = i >= 32
        rows = slice(i * 128, (i + 1) * 128)
        p.dma(X[0][:], xc.ap()[rows, :], w=["X0"], q="sync" if i % 2 == 0 else "gpsimd")
        ln_stats_generic(p, st, X[0], "X0", X[1], "X1", X[2], "X2", 0)
        p.V(lambda e: e.tensor_scalar(out=X[1][:], in0=X[1][:], scalar1=st[:, 1:2], scalar2=None, op0=ALU.mult),
            r=["X1", "st"], w=["X1"])
        for bk in range(2):
            def trh(e, bk=bk):
                ins = None
                for c in range(bk * 4, bk * 4 + 4):
                    ins = e.transpose(psA[:, c * 128:(c + 1) * 128], X[1][:, c * 128:(c + 1) * 128], ident[:])
                return ins
            p.T(trh, r=["X1", "ident"], w=[f"psA{bk}"])
        so = 8 if isctx else 0
        sh = 48 if isctx else 0
        for c in range(8):
            if c < 4:
                p.V(lambda e, c=c, so=so, sh=sh, i=i: e.tensor_scalar(
                    out=hT[:, c, i * 128:(i + 1) * 128], in0=psA[:, c * 128:(c + 1) * 128],
                    scalar1=onep[:, so + c:so + c + 1], scalar2=vT[:, sh + c:sh + c + 1], op0=ALU.mult, op1=ALU.add),
                    r=["onep", "vT"], w=["psA0", ("hT", i)])
            else:
                p.A(lambda e, c=c, so=so, sh=sh, i=i: e.activation(
                    out=hT[:, c, i * 128:(i + 1) * 128], in_=psA[:, c * 128:(c + 1) * 128], func=AF.Identity,
                    scale=onep[:, so + c:so + c + 1], bias=vT[:, sh + c:sh + c + 1]),
                    r=["onep", "vT"], w=["psA1", ("hT", i)])
    k = 0
    for (c0, M) in F_GROUPS:
        for (t0, N) in TOK_GROUPS:
            b = k % 4
            k += 1

            def mm(e, c0=c0, M=M, t0=t0, N=N, b=b):
                ins = None
                for c in range(8):
                    ins = e.matmul(psB[b][0:M, 0:N], lhsT=wF_s[:, c, c0:c0 + M], rhs=hT[:, c, t0:t0 + N],
                                   start=(c == 0), stop=(c == 7))
                return ins
            p.T(mm, r=["wF_s"] + [("hT", t0 // 128 + q) for q in range(N // 128)], w=[f"psB{b}"])
            if k % 2 == 0:
                p.V(lambda e, M=M, N=N, b=b: e.tensor_copy(out=stg[b][0:M, 0:N], in_=psB[b][0:M, 0:N]),
                    w=[f"psB{b}", f"stg{b}"])
            else:
                p.A(lambda e, M=M, N=N, b=b: e.copy(out=stg[b][0:M, 0:N], in_=psB[b][0:M, 0:N]),
                    w=[f"psB{b}", f"stg{b}"])
            p.dma(outF.ap()[c0:c0 + M, t0:t0 + N], stg[b][0:M, 0:N], r=[f"stg{b}"],
                  q="sync" if k % 2 == 0 else "gpsimd")
    for i in range(NTA):
        for (c0, n) in [(0, 512), (512, TCOLS - 512)]:
            b = k % 4
            k += 1

            def mm(e, c0=c0, n=n, i=i, b=b):
                ins = None
                for c in range(8):
                    ins = e.matmul(psB[b][:, 0:n], lhsT=hT[:, c, i * 128:(i + 1) * 128], rhs=wT_s[:, c, c0:c0 + n],
                                   start=(c == 0), stop=(c == 7))
                return ins
            p.T(mm, r=["wT_s", ("hT", i)], w=[f"psB{b}"])
            if k % 2 == 0:
                p.V(lambda e, n=n, b=b: e.tensor_copy(out=stg[b][:, 0:n], in_=psB[b][:, 0:n]),
                    w=[f"psB{b}", f"stg{b}"])
            else:
                p.A(lambda e, n=n, b=b: e.copy(out=stg[b][:, 0:n], in_=psB[b][:, 0:n]), w=[f"psB{b}", f"stg{b}"])
            p.dma(outT.ap()[i * 128:(i + 1) * 128, c0:c0 + n], stg[b][:, 0:n], r=[f"stg{b}"],
                  q="sync" if k % 2 == 0 else "gpsimd")
    p.end_stage()


def emit_A5(p):
    glaT = p.dram("glaT", [128, TA])
    naT = p.dram("naT", [2, 64, TA])
    gdnT = p.dram("gdnT", [2, 128, TA])
    wo = p.dram("wo", [512, D])
    part = p.dram("part", [TA, D], kind="ExternalOutput")
    mix = p.sb("mix", [128, 4, TA], BF16)
    wo_s = p.sb("wo_s", [128, 4, D], BF16)
    stg = [p.sb(f"stg{i}", [128, 512]) for i in range(4)]
    psB = [p.ps(f"psB{i}", [128, 512]) for i in range(4)]
    p.dma(mix[:, 0, :], glaT.ap(), w=["mix"], q="gpsimd")
    p.dma(mix[0:64, 1, :], naT.ap()[0], w=["mix"], q="gpsimd")
    p.dma(mix[64:128, 1, :], naT.ap()[1], w=["mix"], q="gpsimd")
    p.dma(mix[:, 2, :], gdnT.ap()[0], w=["mix"], q="gpsimd")
    p.dma(mix[:, 3, :], gdnT.ap()[1], w=["mix"], q="gpsimd")
    p.dma(wo_s[:], wo.ap().rearrange("(c p) d -> p c d", p=128), w=["wo_s"], q="gpsimd")
    k = 0
    for i in range(NTA):
        for dh in range(2):
            b = k % 4
            k += 1

            def mm(e, i=i, dh=dh, b=b):
                ins = None
                for c in range(4):
                    ins = e.matmul(psB[b][:, :], lhsT=mix[:, c, i * 128:(i + 1) * 128],
                                   rhs=wo_s[:, c, dh * 512:(dh + 1) * 512], start=(c == 0), stop=(c == 3))
                return ins
            p.T(mm, r=["mix", "wo_s"], w=[f"psB{b}"])
            if k % 2 == 0:
                p.V(lambda e, b=b: e.tensor_copy(out=stg[b][:], in_=psB[b][:]), w=[f"psB{b}", f"stg{b}"])
            else:
                p.A(lambda e, b=b: e.copy(out=stg[b][:], in_=psB[b][:]), w=[f"psB{b}", f"stg{b}"])
            p.dma(part.ap()[i * 128:(i + 1) * 128, dh * 512:(dh + 1) * 512], stg[b][:], r=[f"stg{b}"],
                  q="sync" if k % 2 == 0 else "gpsimd")
    p.end_stage()


def core_w_in(w_in_l, hh):
    c = lambda a, n: w_in_l[:, a:a + n]
    F = [c(0 + hh * 64, 32), c(0 + hh * 64 + 32, 32), c(128 + hh * 64, 32), c(128 + hh * 64 + 32, 32), c(512, 32),
         c(1056 + hh * 128, 128), c(1568 + hh * 256, 256), c(2080 + hh * 256, 256), c(2592 + hh * 256, 256)]
    bl = [w_in_l[:, 3104 + d * 4 + 2 * hh + j:3104 + d * 4 + 2 * hh + j + 1] for d in range(2) for j in range(2)]
    al = [w_in_l[:, 3112 + d * 4 + 2 * hh + j:3112 + d * 4 + 2 * hh + j + 1] for d in range(2) for j in range(2)]
    T = [c(256 + hh * 128, 128), c(544 + hh * 128, 128), c(800 + hh * 128, 128), c(1312 + hh * 128, 128),
         c(3120 + hh * 256, 256)] + bl + al
    return np.ascontiguousarray(np.concatenate(F, 1)), np.ascontiguousarray(np.concatenate(T, 1))


NEG = -30000.0


def na_consts():
    jblk = np.zeros((128, 128), np.float32)
    for p_ in range(128):
        jblk[p_, (p_ // 64) * 64 + 63 - (p_ % 64)] = 1.0
    mask = np.full((64, 64), NEG, np.float32)
    for qcp in range(64):
        qc = 63 - qcp
        c0 = min(max(qc - 8, 0), 48)
        mask[c0:c0 + 16, qcp] = 0.0
    return jblk, np.concatenate([mask, mask], 0)


def emit_A2(p):
    outF = p.dram("outF", [FROWS, TA])
    outT = p.dram("outT", [TA, TCOLS])
    rpb = p.dram("rpb", [2, 15, 127])
    ident_d = p.dram("ident", [128, 128])
    jblk_d = p.dram("jblk", [128, 128])
    mask_d = p.dram("mask2", [128, 64])
    naT = p.dram("naT", [2, 64, TA], kind="ExternalOutput")
    identb = p.sb("identb", [128, 128], BF16)
    jblkb = p.sb("jblkb", [128, 128], BF16)
    mask2 = p.sb("mask2s", [128, 64])
    kT = p.sb("kT", [64, 2, TA], BF16)
    qT = p.sb("qT", [64, 2, TA], BF16)
    vst = p.sb("vst", [128, NTA, 128], BF16)
    qst = p.sb("qst", [128, NTA, 128], BF16)
    v_sb = p.sb("v_sb", [128, NTA, 2, 65], BF16)
    Bst = p.sb("Bst", [128, 2, 16, 64])
    Bm = p.sb("Bm", [128, 2, 16, 64], BF16)
    PT = [p.sb(f"PT{i}", [128, 7, 64], BF16) for i in range(2)]
    on = [p.sb(f"on{i}", [64, 64], BF16) for i in range(2)]
    rec = [p.sb(f"rec{i}", [64, 1]) for i in range(2)]
    osb = p.sb("osb", [64, 2, TA])
    pS = [p.ps(f"pS{i}", [128, 512]) for i in range(2)]
    pO = [p.ps(f"pO{i}", [128, 512]) for i in range(2)]
    pF = [p.ps(f"pF{i}", [128, 512]) for i in range(2)]
    pQ = [p.ps(f"pQ{i}", [128, 512]) for i in range(2)]
    p.dma(identb[:], ident_d.ap(), w=["identb"], q="gpsimd")
    p.dma(jblkb[:], jblk_d.ap(), w=["jblkb"], q="gpsimd")
    p.dma(mask2[:], mask_d.ap(), w=["mask2"])
    for j in range(2):
        p.dma(kT[:, j, :], outF.ap()[160 + j * 64:160 + (j + 1) * 64, :], w=["kT"], q="gpsimd")
    p.dma(vst[:], outT.ap()[:, 384:512].rearrange("(n p) c -> p n c", p=128), w=["vst"], q="gpsimd")
    p.dma(qst[:], outT.ap()[:, 256:384].rearrange("(n p) c -> p n c", p=128), w=["qst"], q="gpsimd")
    p.V(lambda e: e.memset(v_sb[:], 1.0), w=["v_sb"])
    for j in range(2):
        p.V(lambda e, j=j: e.tensor_copy(out=v_sb[:, :, j, 0:64], in_=vst[:, :, j * 64:(j + 1) * 64]),
            r=["vst"], w=["v_sb"])
    p.V(lambda e: e.memset(Bst[:], 0.0), w=["Bst"])
    for j in range(2):
        base = j * 15 * 127
        p.dma(Bst[0:64, j, 0:14, :], bass.AP(tensor=rpb, offset=base, ap=[[1, 64], [127, 14], [1, 64]]), w=["Bst"])
        p.dma(Bst[64:128, j, 0:14, :], bass.AP(tensor=rpb, offset=base + 127, ap=[[1, 64], [127, 14], [1, 64]]),
              w=["Bst"])
        p.dma(Bst[64:128, j, 14, :], bass.AP(tensor=rpb, offset=base + 3 * 127, ap=[[1, 64], [1, 64]]), w=["Bst"])
        p.dma(Bst[0:64, j, 15, :], bass.AP(tensor=rpb, offset=base + 10 * 127, ap=[[1, 64], [1, 64]]), w=["Bst"])
    for j in range(2):
        for m in range(16):
            p.V(lambda e, j=j, m=m: e.tensor_tensor(out=Bm[:, j, m, :], in0=Bst[:, j, m, :], in1=mask2[:], op=ALU.add),
                r=["Bst", "mask2"], w=["Bm"])
        p.V(lambda e, j=j: e.memset(Bm[0:64, j, 14, :], NEG), w=["Bm"])
        p.V(lambda e, j=j: e.memset(Bm[64:128, j, 15, :], NEG), w=["Bm"])
    k = 0
    for i in range(NTA):
        for j in range(2):
            b = k % 2
            k += 1
            rhs = identb if i >= 32 else jblkb
            p.T(lambda e, i=i, j=j, b=b, rhs=rhs: e.matmul(pQ[b][0:64, 0:128], lhsT=qst[:, i, j * 64:(j + 1) * 64],
                                                           rhs=rhs[:], start=True, stop=True),
                r=["qst", "identb", "jblkb"], w=[f"pQ{b}"])
            p.A(lambda e, i=i, j=j, b=b: e.mul(out=qT[:, j, i * 128:(i + 1) * 128], in_=pQ[b][0:64, 0:128], mul=0.125),
                w=[f"pQ{b}", ("qT", i)])
    k = 0
    for r in range(68):
        if r < 64:
            r0 = min(max(r - 4, 0), 56)
            tiles = list(range(r0 // 2, (r0 + 7) // 2 + 1))
            chunks = []
            for t in tiles:
                lo, hi = 2 * t, 2 * t + 1
                in_lo = r0 <= lo < r0 + 8
                in_hi = r0 <= hi < r0 + 8
                if in_lo and in_hi:
                    slot = lo - r + 7
                    assert 0 <= slot <= 13
                elif in_hi:
                    assert hi - r == -4
                    slot = 14
                else:
                    assert in_lo and lo - r == 3
                    slot = 15
                chunks.append((t, slot, slot))
            chunks += [(32, None, None), (33, None, None)]
        else:
            chunks = [(32, None, None), (33, None, None)]
        nch = len(chunks)
        for j in range(2):
            b = k % 2
            k += 1

            def qk(e, r=r, j=j, b=b, chunks=chunks):
                ins = None
                for ci, (t, s_lo, s_hi) in enumerate(chunks):
                    o_ = pS[b][:, ci * 64:(ci + 1) * 64]
                    ins = e.matmul(o_, lhsT=kT[:, j, t * 128:(t + 1) * 128], rhs=qT[:, j, r * 64:(r + 1) * 64],
                                   start=True, stop=(s_lo is None))
                    if s_lo is not None:
                        ins = e.matmul(o_, lhsT=identb[:, :], rhs=Bm[:, j, s_lo, :], start=False, stop=True)
                return ins
            p.T(qk, r=["kT", ("qT", r // 2), "identb", "Bm"], w=[f"pS{b}"])
            p.A(lambda e, b=b, nch=nch: e.activation(out=PT[b][:, 0:nch, :],
                                                     in_=pS[b][:, 0:nch * 64].rearrange("p (c q) -> p c q", q=64),
                                                     func=AF.Exp), w=[f"pS{b}", f"PT{b}"])

            def pv(e, j=j, b=b, chunks=chunks):
                ins = None
                n_ = len(chunks)
                for ci, (t, _, _) in enumerate(chunks):
                    ins = e.matmul(pO[b][0:64, 0:65], lhsT=PT[b][:, ci, :], rhs=v_sb[:, t, j, :], start=(ci == 0),
                                   stop=(ci == n_ - 1))
                return ins
            p.T(pv, r=[f"PT{b}", "v_sb"], w=[f"pO{b}"])
            p.V(lambda e, b=b: e.reciprocal(out=rec[b][:], in_=pO[b][0:64, 64:65]), w=[f"pO{b}", f"rec{b}"])
            p.V(lambda e, b=b: e.tensor_scalar(out=on[b][:], in0=pO[b][0:64, 0:64], scalar1=rec[b][:, 0:1], scalar2=None,
                                               op0=ALU.mult), r=[f"rec{b}"], w=[f"pO{b}", f"on{b}"])
            rhs = identb if r >= 64 else jblkb
            p.T(lambda e, b=b, rhs=rhs: e.matmul(pF[b][0:64, 0:64], lhsT=on[b][:], rhs=rhs[0:64, 0:64], start=True,
                                                 stop=True), r=[f"on{b}", "identb", "jblkb"], w=[f"pF{b}"])
            p.A(lambda e, b=b, j=j, r=r: e.copy(out=osb[:, j, r * 64:(r + 1) * 64], in_=pF[b][0:64, 0:64]),
                w=[f"pF{b}", "osb"])
    for j in range(2):
        p.dma(naT.ap()[j], osb[:, j, :], r=["osb"])
    p.end_stage()


def gla_consts():
    s = np.arange(128)[:, None]
    t = np.arange(128)[None, :]
    c = -1.0 / 16.0
    tri = np.stack([(s <= t) * c, (s >= t) * c, (s > t) * c, (s < t) * c]).astype(np.float32)
    msk = np.stack([(s <= t), (s >= t)]).astype(np.float32)
    tok = np.arange(TA)
    half = 16
    inv_freq = (10000.0 ** (-np.arange(0, half, 2, dtype=np.float32) / half)).astype(np.float32)
    cos = np.ones((32, TA), np.float32)
    sin = np.zeros((32, TA), np.float32)
    lat = tok[:4096]
    for i in range(32):
        blk, within = i // 16, i % 16
        f = within % 8
        pos = (lat // 64 if blk == 0 else lat % 64).astype(np.float32)
        ang = pos * inv_freq[f]
        cos[i, :4096] = np.cos(ang)
        sin[i, :4096] = np.sin(ang)
    P = np.zeros((32, 32), np.float32)
    for i in range(32):
        within = i % 16
        if within < 8:
            P[i, i + 8] = -1.0
        else:
            P[i, i - 8] = 1.0
    return tri, msk, cos, sin, np.ascontiguousarray(P.T)


SCAN_F = [32, 33] + list(range(32))
SCAN_B = [33, 32] + list(range(31, -1, -1))


def emit_A3(p):
    outF = p.dram("outF", [FROWS, TA])
    outT = p.dram("outT", [TA, TCOLS])
    wz_d = p.dram("wz", [33, 128])
    gn_d = p.dram("gn", [1, 64])
    ident_d = p.dram("ident", [128, 128])
    tri_d = p.dram("tri", [4, 128, 128])
    msk_d = p.dram("msk", [2, 128, 128])
    cos_d = p.dram("cos", [32, TA])
    sin_d = p.dram("sin", [32, TA])
    pt_d = p.dram("ptr", [32, 32])
    glaT = p.dram("glaT", [128, TA], kind="ExternalOutput")
    ident = p.sb("ident_s", [128, 128])
    tri = p.sb("tri_s", [128, 4, 128])
    msk = p.sb("msk_s", [128, 2, 128])
    ptr = p.sb("ptr_s", [32, 32])
    wz = p.sb("wz_s", [33, 128])
    gn = p.sb("gn_s", [128, 64])
    cs = p.sb("cos_s", [32, TA])
    sn = p.sb("sin_s", [32, TA])
    qr = p.sb("qr", [32, 2, TA])
    kr = p.sb("kr", [32, 2, TA])
    aT = p.sb("aT", [33, TA])
    v_bf = p.sb("v_bf", [128, NTA, 128], BF16)
    g_sb = p.sb("g_sb", [128, NTA, 128])
    o_acc = p.sb("o_acc", [128, NTA, 128])
    osb = p.sb("osb", [128, TA])
    S = [p.sb(f"S{d}", [32, 2, 64]) for d in range(2)]
    Sbf = [p.sb(f"Sbf{d}", [32, 2, 64], BF16) for d in range(2)]
    tmp = [p.sb(f"tmp{i}", [32, 512]) for i in range(2)]
    ez = [p.sb(f"ez{d}", [128, 64]) for d in range(2)]
    L = [p.sb(f"L{d}", [128, 64]) for d in range(2)]
    E = [p.sb(f"E{d}", [32, 256]) for d in range(2)]
    Ei = [p.sb(f"Ei{d}", [32, 256]) for d in range(2)]
    qd = [p.sb(f"qd{d}", [32, 2, 128], BF16) for d in range(2)]
    ki = [p.sb(f"ki{d}", [32, 2, 128], BF16) for d in range(2)]
    Ed = [p.sb(f"Ed{d}", [128, 64]) for d in range(2)]
    kdec = [p.sb(f"kdec{d}", [128, 64], BF16) for d in range(2)]
    attn = [p.sb(f"attn{d}", [128, 2, 128], BF16) for d in range(2)]
    st = p.sb("st", [128, 8])
    sg = p.sb("sg", [128, 128])
    tt = p.sb("tt", [128, 128])
    junk = p.sb("junk", [128, 64])
    PS = [p.ps(f"ps{i}", [128, 512]) for i in range(8)]
    pz, pg, pe, pk, pa, po, pkv, pr = PS

    p.dma(ident[:], ident_d.ap(), w=["ident"])
    p.dma(tri[:], tri_d.ap().rearrange("a s t -> s a t"), w=["tri"])
    p.dma(msk[:], msk_d.ap().rearrange("a s t -> s a t"), w=["msk"])
    p.dma(ptr[:], pt_d.ap(), w=["ptr"])
    p.dma(wz[:], wz_d.ap(), w=["wz"])
    p.dma(gn[:], gn_d.ap().partition_broadcast(128), w=["gn"])
    p.dma(cs[:], cos_d.ap(), w=["cs"])
    p.dma(sn[:], sin_d.ap(), w=["sn"], q="gpsimd")
    for j in range(2):
        p.dma(qr[:, j, :], outF.ap()[j * 32:(j + 1) * 32, :], w=[("qr", j)])
        p.dma(kr[:, j, :], outF.ap()[64 + j * 32:64 + (j + 1) * 32, :], w=[("kr", j)], q="gpsimd")
    p.dma(aT[0:32, :], outF.ap()[128:160, :], w=["aT"])
    p.V(lambda e: e.memset(aT[32:33, :], 1.0), w=["aT1"])
    p.dma(v_bf[:], outT.ap()[:, 0:128].rearrange("(n p) c -> p n c", p=128), w=["v_bf"], q="gpsimd")
    p.dma(g_sb[:], outT.ap()[:, 128:256].rearrange("(n p) c -> p n c", p=128), w=["g_sb"])
    p.G(lambda e: e.memset(o_acc[:], 0.0), w=["o_acc"])
    for d in range(2):
        p.V(lambda e, d=d: e.memset(S[d][:], 0.0), w=[f"S{d}"])
        p.V(lambda e, d=d: e.memset(Sbf[d][:], 0.0), w=[f"Sbf{d}"])
    k = 0
    for (X, xn) in ((qr, "qr"), (kr, "kr")):
        for j in range(2):
            for (t0, N) in TOK_GROUPS:
                b = k % 2
                k += 1
                xs = X[:, j, t0:t0 + N]
                p.T(lambda e, xs=xs, N=N: e.matmul(pr[0:32, 0:N], lhsT=ptr[:, :], rhs=xs, start=True, stop=True),
                    r=["ptr", (xn, j)], w=["pr"])
                p.V(lambda e, b=b, t0=t0, N=N: e.tensor_tensor(out=tmp[b][:, 0:N], in0=pr[0:32, 0:N],
                                                               in1=sn[:, t0:t0 + N], op=ALU.mult),
                    r=["sn"], w=["pr", f"tmp{b}"])
                p.G(lambda e, xs=xs, t0=t0, N=N: e.tensor_tensor(out=xs, in0=xs, in1=cs[:, t0:t0 + N], op=ALU.mult),
                    r=["cs"], w=[(xn, j)])
                p.G(lambda e, xs=xs, b=b, N=N: e.tensor_tensor(out=xs, in0=xs, in1=tmp[b][:, 0:N], op=ALU.add),
                    r=[f"tmp{b}"], w=[(xn, j)])
    for step in range(NTA):
        for d in range(2):
            n = (SCAN_F if d == 0 else SCAN_B)[step]
            ts = slice(n * 128, (n + 1) * 128)
            inc, exc = (0, 2) if d == 0 else (1, 3)
            p.T(lambda e, ts=ts, d=d: e.matmul(pz[:, 0:64], lhsT=aT[0:33, ts], rhs=wz[0:33, d * 64:(d + 1) * 64],
                                               start=True, stop=True), r=["aT", "aT1", "wz"], w=["pz"])
            p.A(lambda e, d=d: e.activation(out=ez[d][:], in_=pz[:, 0:64], func=AF.Exp, scale=-1.0),
                w=["pz", f"ez{d}"])
            p.A(lambda e, d=d: e.activation(out=L[d][:], in_=ez[d][:], func=AF.Ln, bias=1.0),
                r=[f"ez{d}"], w=[f"L{d}"])

            def mg(e, d=d, inc=inc):
                ins = None
                for j in range(2):
                    ins = e.matmul(pg[0:32, j * 128:(j + 1) * 128], lhsT=L[d][:, j * 32:(j + 1) * 32],
                                   rhs=tri[:, inc, :], start=True, stop=True)
                return ins
            p.T(mg, r=[f"L{d}", "tri"], w=["pg"])
            p.A(lambda e, d=d: e.activation(out=E[d][:], in_=pg[0:32, 0:256], func=AF.Exp), w=["pg", f"E{d}"])
            p.A(lambda e, d=d: e.activation(out=Ei[d][:], in_=pg[0:32, 0:256], func=AF.Exp, scale=-1.0),
                w=["pg", f"Ei{d}"])
            p.V(lambda e, d=d, ts=ts: e.scalar_tensor_tensor(
                out=qd[d][:], in0=qr[:, :, ts], scalar=32.0 ** -0.5,
                in1=E[d][:].rearrange("p (j t) -> p j t", j=2), op0=ALU.mult, op1=ALU.mult),
                r=[("qr", 0), ("qr", 1), f"E{d}"], w=[f"qd{d}"])
            p.G(lambda e, d=d, ts=ts: e.tensor_tensor(out=ki[d][:], in0=kr[:, :, ts],
                                                      in1=Ei[d][:].rearrange("p (j t) -> p j t", j=2), op=ALU.mult),
                r=[("kr", 0), ("kr", 1), f"Ei{d}"], w=[f"ki{d}"])
            p.T(lambda e, d=d, exc=exc: e.matmul(pe[:, 0:64], lhsT=tri[:, exc, :], rhs=L[d][:], start=True, stop=True),
                r=["tri", f"L{d}"], w=["pe"])
            p.A(lambda e, d=d: e.activation(out=Ed[d][:], in_=pe[:, 0:64], func=AF.Exp), w=["pe", f"Ed{d}"])

            def tk(e, ts=ts):
                ins = None
                for j in range(2):
                    ins = e.transpose(pk[:, j * 32:(j + 1) * 32], kr[:, j, ts], ident[0:32, 0:32])
                return ins
            p.T(tk, r=[("kr", 0), ("kr", 1), "ident"], w=["pk"])
            p.V(lambda e, d=d: e.tensor_tensor(out=kdec[d][:], in0=pk[:, 0:64], in1=Ed[d][:], op=ALU.mult),
                r=[f"Ed{d}"], w=["pk", f"kdec{d}"])

            def ma(e, d=d):
                ins = None
                for j in range(2):
                    ins = e.matmul(pa[:, j * 128:(j + 1) * 128], lhsT=ki[d][:, j, :], rhs=qd[d][:, j, :], start=True,
                                   stop=True)
                return ins
            p.T(ma, r=[f"ki{d}", f"qd{d}"], w=["pa"])
            for j in range(2):
                p.V(lambda e, d=d, j=j: e.tensor_tensor(out=attn[d][:, j, :], in0=pa[:, j * 128:(j + 1) * 128],
                                                        in1=msk[:, d, :], op=ALU.mult),
                    r=["msk"], w=["pa", (f"attn{d}", j)])

            def mo(e, d=d, n=n):
                ins = None
                for j in range(2):
                    e.matmul(po[:, j * 64:(j + 1) * 64], lhsT=attn[d][:, j, :], rhs=v_bf[:, n, j * 64:(j + 1) * 64],
                             start=True, stop=False)
                    ins = e.matmul(po[:, j * 64:(j + 1) * 64], lhsT=qd[d][:, j, :], rhs=Sbf[d][:, j, :], start=False,
                                   stop=True)
                return ins
            p.T(mo, r=[(f"attn{d}", 0), (f"attn{d}", 1), "v_bf", f"qd{d}", f"Sbf{d}"], w=["po"])
            p.V(lambda e, n=n: e.tensor_tensor(out=o_acc[:, n, :], in0=po[:, 0:128], in1=o_acc[:, n, :], op=ALU.add),
                w=["po", ("o_acc", n), "o_acc"] if False else ["po", "o_acc"])

            def mkv(e, d=d, n=n):
                ins = None
                for j in range(2):
                    ins = e.matmul(pkv[0:32, j * 64:(j + 1) * 64], lhsT=kdec[d][:, j * 32:(j + 1) * 32],
                                   rhs=v_bf[:, n, j * 64:(j + 1) * 64], start=True, stop=True)
                return ins
            p.T(mkv, r=[f"kdec{d}", "v_bf"], w=["pkv"])
            lastc = 127 if d == 0 else 0
            for j in range(2):
                p.V(lambda e, d=d, j=j, lastc=lastc: e.scalar_tensor_tensor(
                    out=S[d][:, j, :], in0=S[d][:, j, :], scalar=E[d][:, j * 128 + lastc:j * 128 + lastc + 1],
                    in1=pkv[0:32, j * 64:(j + 1) * 64], op0=ALU.mult, op1=ALU.add),
                    r=[f"E{d}"], w=["pkv", f"S{d}"])
            p.A(lambda e, d=d: e.copy(out=Sbf[d][:], in_=S[d][:]), r=[f"S{d}"], w=[f"Sbf{d}"])
    for n in range(NTA):
        for j in range(2):
            p.A(lambda e, n=n, j=j: e.activation(out=junk[:], in_=o_acc[:, n, j * 64:(j + 1) * 64], func=AF.Square,
                                                 accum_out=st[:, j:j + 1]), r=["o_acc"], w=["junk", "st"])
        p.V(lambda e: e.tensor_scalar(out=st[:, 2:4], in0=st[:, 0:2], scalar1=1.0 / 64, scalar2=1e-6, op0=ALU.mult,
                                      op1=ALU.add), r=["st"], w=["st"])
        p.A(lambda e: e.activation(out=st[:, 2:4], in_=st[:, 2:4], func=AF.Sqrt), r=["st"], w=["st"])
        p.V(lambda e: e.reciprocal(out=st[:, 4:6], in_=st[:, 2:4]), r=["st"], w=["st"])
        p.A(lambda e, n=n: e.activation(out=sg[:], in_=g_sb[:, n, :], func=AF.Silu), r=["g_sb"], w=["sg"])
        for j in range(2):
            p.V(lambda e, n=n, j=j: e.scalar_tensor_tensor(
                out=tt[:, j * 64:(j + 1) * 64], in0=o_acc[:, n, j * 64:(j + 1) * 64], scalar=st[:, 4 + j:5 + j],
                in1=gn[:], op0=ALU.mult, op1=ALU.mult), r=["o_acc", "st", "gn"], w=["tt"])
        p.V(lambda e: e.tensor_tensor(out=tt[:], in0=tt[:], in1=sg[:], op=ALU.mult), r=["sg"], w=["tt"])
        p.T(lambda e: e.transpose(pr[:, 0:128], tt[:], ident[:]), r=["tt", "ident"], w=["pr"])
        p.A(lambda e, n=n: e.copy(out=osb[:, n * 128:(n + 1) * 128], in_=pr[:, 0:128]), w=["pr", "osb"])
    p.dma(glaT.ap(), osb[:], r=["osb"])
    p.end_stage()


def core_wz(gla_w_a2_l, gla_b_a_l, hh):
    wz = np.zeros((33, 128), np.float32)
    for d in range(2):
        for j in range(2):
            h = 2 * hh + j
            wz[d * 16:(d + 1) * 16, d * 64 + j * 32:d * 64 + (j + 1) * 32] = gla_w_a2_l[d, :, h * 32:(h + 1) * 32]
            wz[32, d * 64 + j * 32:d * 64 + (j + 1) * 32] = gla_b_a_l[d, h * 32:(h + 1) * 32]
    return wz


def gdn_consts():
    t = np.arange(128)[:, None]
    s = np.arange(128)[None, :]
    tri = np.stack([(t <= s), (t >= s), (t > s), (t < s)]).astype(np.float32)
    negm = np.stack([np.where(s <= t, 0.0, NEG), np.where(s >= t, 0.0, NEG)]).astype(np.float32)
    strict = np.stack([(s < t), (s > t)]).astype(np.float32)
    return tri, negm, strict


SEQS = [(0, 4096), (4096, TA)]


def emit_A4(p, n_steps=NTA):
    outF = p.dram("outF", [FROWS, TA])
    outT = p.dram("outT", [TA, TCOLS])
    cw_d = p.dram("cw", [128, 30])
    ab_d = p.dram("ab", [1, 8])
    gn_d = p.dram("ggn", [1, 128])
    ident_d = p.dram("ident", [128, 128])
    tri_d = p.dram("gtri", [4, 128, 128])
    negm_d = p.dram("negm", [2, 128, 128])
    str_d = p.dram("strict", [2, 128, 128])
    gdnT = p.dram("gdnT", [2, 128, TA], kind="ExternalOutput")
    ident = p.sb("ident_s", [128, 128])
    identb = p.sb("identb", [128, 128], BF16)
    ones = p.sb("ones", [128, 128])
    nones = p.sb("nones", [128, 128])
    tri = p.sb("tri_s", [128, 4, 128])
    negm = p.sb("negm_s", [128, 2, 128])
    strict = p.sb("strict_s", [128, 2, 128])
    cw = p.sb("cw_s", [128, 30])
    ab = p.sb("ab_s", [128, 8])
    negA = p.sb("negA", [128, 4])
    gn = p.sb("gn_s", [128, 128])
    xb = p.sb("xb", [128, TA])
    acc = p.sb("accb", [128, TA])
    rn = [p.sb(f"rn{i}", [128, 512]) for i in range(2)]
    qn = p.sb("qn", [128, 2, TA], BF16)
    kn = p.sb("kn", [128, 2, TA], BF16)
    vc = p.sb("vc", [128, 2, TA], BF16)
    ba = p.sb("ba", [128, NTA, 8])
    beta = p.sb("beta", [128, NTA, 4])
    nbeta = p.sb("nbeta", [128, NTA, 4])
    sp = p.sb("sp", [128, NTA, 4])
    g_all = p.sb("g_all", [128, NTA, 4])
    o_acc = p.sb("o_acc", [128, NTA, 256])
    osb = p.sb("osb", [128, 2, TA])
    S = [[p.sb(f"S{d}{j}", [128, 128]) for j in range(2)] for d in range(2)]
    Sbf = [[p.sb(f"Sbf{d}{j}", [128, 128], BF16) for j in range(2)] for d in range(2)]

    def T32(nm):
        return [p.sb(f"{nm}{d}", [128, 128]) for d in range(2)]

    def T16(nm):
        return [p.sb(f"{nm}{d}", [128, 128], BF16) for d in range(2)]
    Gt, dm, dec, t1, CT, qk, Csb = T32("Gt"), T32("dm"), T32("dec"), T32("t1"), T32("CT"), T32("qk"), T32("Csb")
    Xa, Xb_, XTa, XTb, Pa, Pb, PTa, PTb = (T32("Xa"), T32("Xb"), T32("XTa"), T32("XTb"), T32("Pa"), T32("Pb"),
                                           T32("PTa"), T32("PTb"))
    rhs_w, rhs_u, u_sb = T32("rhsw"), T32("rhsu"), T32("usb")
    qkT, wT, kdec, vnew = T16("qkT"), T16("wT"), T16("kdec"), T16("vnew")
    E3 = [p.sb(f"E3{d}", [128, 4]) for d in range(2)]
    st = p.sb("st", [128, 8])
    sg = p.sb("sg", [128, 256])
    tt = p.sb("tt", [128, 256])
    zt = p.sb("zt", [128, 256])
    junk = p.sb("junk", [128, 128])
    PS = [p.ps(f"ps{i}", [128, 512]) for i in range(6)]
    pD, p3, pK, pTr, pX, pP = PS
    pT2 = p.ps("pT2", [128, 1024], BF16)
    pO = p.ps("pO", [128, 512])
    BK = {"pD": "ps0", "p3": "ps1", "pK": "ps2", "pTr": "ps3", "pX": "ps4", "pP": "ps5", "pT2": "pT2", "pO": "pO"}

    p.dma(ident[:], ident_d.ap(), w=["ident"])
    p.dma(identb[:], ident_d.ap(), w=["identb"], q="gpsimd")
    p.dma(tri[:], tri_d.ap().rearrange("a s t -> s a t"), w=["tri"])
    p.dma(negm[:], negm_d.ap().rearrange("a s t -> s a t"), w=["negm"])
    p.dma(strict[:], str_d.ap().rearrange("a s t -> s a t"), w=["strict"])
    p.dma(cw[:], cw_d.ap(), w=["cw"])
    p.dma(ab[:], ab_d.ap().partition_broadcast(128), w=["ab"])
    p.dma(gn[:], gn_d.ap().partition_broadcast(128), w=["gn"])
    p.dma(ba[:], outT.ap()[:, 768:776].rearrange("(n p) c -> p n c", p=128), w=["ba"])
    p.V(lambda e: e.memset(ones[:], 1.0), w=["ones"])
    p.V(lambda e: e.memset(nones[:], -1.0), w=["nones"])
    p.G(lambda e: e.memset(o_acc[:], 0.0), w=["o_acc"])
    for d in range(2):
        for j in range(2):
            p.V(lambda e, d=d, j=j: e.memset(S[d][j][:], 0.0), w=[f"S{d}{j}"])
            p.V(lambda e, d=d, j=j: e.memset(Sbf[d][j][:], 0.0), w=[f"Sbf{d}{j}"])
    p.A(lambda e: e.activation(out=negA[:], in_=ab[:, 0:4], func=AF.Exp), r=["ab"], w=["negA"])
    p.V(lambda e: e.tensor_scalar(out=negA[:], in0=negA[:], scalar1=-1.0, scalar2=None, op0=ALU.mult),
        r=["negA"], w=["negA"])
    p.A(lambda e: e.activation(out=beta[:], in_=ba[:, :, 0:4], func=AF.Sigmoid), r=["ba"], w=["beta"])
    p.V(lambda e: e.tensor_scalar(out=nbeta[:], in0=beta[:], scalar1=-1.0, scalar2=None, op0=ALU.mult),
        r=["beta"], w=["nbeta"])
    for c in range(4):
        p.A(lambda e, c=c: e.activation(out=sp[:, :, c], in_=ba[:, :, 4 + c], func=AF.Exp, bias=ab[:, 4 + c:5 + c]),
            r=["ba", "ab"], w=["sp"])
    p.A(lambda e: e.activation(out=sp[:], in_=sp[:], func=AF.Ln, bias=1.0), r=["sp"], w=["sp"])
    for c in range(4):
        p.V(lambda e, c=c: e.tensor_scalar(out=g_all[:, :, c], in0=sp[:, :, c], scalar1=negA[:, c:c + 1], scalar2=None,
                                           op0=ALU.mult), r=["sp", "negA"], w=["g_all"])
    grp_rows = [288, 416, 544, 672, 800, 928]
    dsts = [(qn, 0), (qn, 1), (kn, 0), (kn, 1), (vc, 0), (vc, 1)]
    kk = 0
    for gi in range(6):
        p.dma(xb[:], outF.ap()[grp_rows[gi]:grp_rows[gi] + 128, :], w=["xb"], q="sync" if gi % 2 == 0 else "gpsimd")
        W = lambda tau, gi=gi: cw[:, gi * 5 + tau:gi * 5 + tau + 1]
        for (a, b_) in SEQS:
            p.V(lambda e, a=a, b_=b_, W=W: e.tensor_scalar(out=acc[:, a:b_], in0=xb[:, a:b_], scalar1=W(2), scalar2=None,
                                                           op0=ALU.mult), r=["xb", "cw"], w=["acc"])
            for tau, (oa, ob, ia, ib) in ((0, (a + 2, b_, a, b_ - 2)), (1, (a + 1, b_, a, b_ - 1)),
                                          (3, (a, b_ - 1, a + 1, b_)), (4, (a, b_ - 2, a + 2, b_))):
                p.V(lambda e, tau=tau, oa=oa, ob=ob, ia=ia, ib=ib, W=W: e.scalar_tensor_tensor(
                    out=acc[:, oa:ob], in0=xb[:, ia:ib], scalar=W(tau), in1=acc[:, oa:ob], op0=ALU.mult, op1=ALU.add),
                    r=["xb", "cw"], w=["acc"])
        p.A(lambda e: e.activation(out=acc[:], in_=acc[:], func=AF.Silu), w=["acc"])
        dst, j = dsts[gi]
        if gi >= 4:
            p.V(lambda e, dst=dst, j=j: e.tensor_copy(out=dst[:, j, :], in_=acc[:]), r=["acc"], w=[("F", gi)])
            continue
        p.A(lambda e: e.activation(out=xb[:], in_=acc[:], func=AF.Square), r=["acc"], w=["xb"])
        for (t0, N) in TOK_GROUPS:
            b = kk % 2
            kk += 1
            p.T(lambda e, t0=t0, N=N: e.matmul(pD[:, 0:N], lhsT=ones[:], rhs=xb[:, t0:t0 + N], start=True, stop=True),
                r=["ones", "xb"], w=["ps0"])
            p.V(lambda e, b=b, N=N: e.tensor_scalar(out=rn[b][:, 0:N], in0=pD[:, 0:N], scalar1=1e-6, scalar2=None,
                                                    op0=ALU.add), w=["ps0", f"rn{b}"])
            p.A(lambda e, b=b, N=N: e.activation(out=rn[b][:, 0:N], in_=rn[b][:, 0:N], func=AF.Sqrt), w=[f"rn{b}"])
            p.V(lambda e, b=b, N=N: e.reciprocal(out=rn[b][:, 0:N], in_=rn[b][:, 0:N]), w=[f"rn{b}"])
            sc_ = (128.0 ** -0.5) if gi < 2 else 1.0
            p.V(lambda e, b=b, t0=t0, N=N, dst=dst, j=j, sc_=sc_: e.scalar_tensor_tensor(
                out=dst[:, j, t0:t0 + N], in0=acc[:, t0:t0 + N], scalar=sc_, in1=rn[b][:, 0:N], op0=ALU.mult,
                op1=ALU.mult), r=["acc", f"rn{b}"], w=[("F", gi)])
    FQ = lambda j: ("F", j)
    FK = lambda j: ("F", 2 + j)
    FV = lambda j: ("F", 4 + j)
    for step in range(n_steps):
        for d in range(2):
            n = (SCAN_F if d == 0 else SCAN_B)[step]
            ts = slice(n * 128, (n + 1) * 128)
            inc, exc = (0, 2) if d == 0 else (1, 3)
            for j in range(2):
                c = d * 2 + j
                gcol = g_all[:, n, c:c + 1]
                nm = lambda s_, d=d: f"{s_}{d}"
                p.V(lambda e, d=d, inc=inc, gcol=gcol: e.tensor_scalar(out=Gt[d][:], in0=tri[:, inc, :], scalar1=gcol,
                                                                      scalar2=None, op0=ALU.mult),
                    r=["tri", "g_all"], w=[nm("Gt")])

                def mD(e, d=d):
                    e.matmul(pD[:, 0:128], lhsT=Gt[d][:], rhs=ones[:], start=True, stop=False)
                    return e.matmul(pD[:, 0:128], lhsT=nones[:], rhs=Gt[d][:], start=False, stop=True)
                p.T(mD, r=[nm("Gt"), "ones", "nones"], w=["ps0"])

                def m3(e, inc=inc, exc=exc, gcol=gcol):
                    e.matmul(p3[:, 0:1], lhsT=tri[:, inc, :], rhs=gcol, start=True, stop=True)
                    e.matmul(p3[:, 1:2], lhsT=tri[:, exc, :], rhs=gcol, start=True, stop=True)
                    return e.matmul(p3[:, 2:3], lhsT=ones[:], rhs=gcol, start=True, stop=True)
                p.T(m3, r=["tri", "ones", "g_all"], w=["ps1"])
                p.V(lambda e, d=d: e.tensor_tensor(out=dm[d][:], in0=pD[:, 0:128], in1=negm[:, d, :], op=ALU.add),
                    r=["negm"], w=["ps0", nm("dm")])
                p.A(lambda e, d=d: e.activation(out=dec[d][:], in_=dm[d][:], func=AF.Exp), r=[nm("dm")], w=[nm("dec")])
                p.A(lambda e, d=d: e.activation(out=E3[d][:, 0:3], in_=p3[:, 0:3], func=AF.Exp), w=["ps1", nm("E3")])

                def mK(e, j=j, ts=ts):
                    e.matmul(pK[:, 0:128], lhsT=kn[:, j, ts], rhs=kn[:, j, ts], start=True, stop=True)
                    return e.matmul(pK[:, 128:256], lhsT=qn[:, j, ts], rhs=kn[:, j, ts], start=True, stop=True)
                p.T(mK, r=[FQ(j), FK(j)], w=["ps2"])
                p.V(lambda e, d=d: e.tensor_tensor(out=t1[d][:], in0=pK[:, 0:128], in1=dec[d][:], op=ALU.mult),
                    r=[nm("dec")], w=["ps2", nm("t1")])
                p.V(lambda e, d=d, n=n, c=c: e.scalar_tensor_tensor(
                    out=CT[d][:], in0=t1[d][:], scalar=nbeta[:, n, c:c + 1], in1=strict[:, d, :], op0=ALU.mult,
                    op1=ALU.mult), r=[nm("t1"), "nbeta", "strict"], w=[nm("CT")])
                p.V(lambda e, d=d: e.tensor_tensor(out=qk[d][:], in0=pK[:, 128:256], in1=dec[d][:], op=ALU.mult),
                    r=[nm("dec")], w=["ps2", nm("qk")])

                def mTr(e, d=d):
                    e.transpose(pTr[:, 0:128], CT[d][:], ident[:])
                    return e.transpose(pTr[:, 128:256], qk[d][:], ident[:])
                p.T(mTr, r=[nm("CT"), nm("qk"), "ident"], w=["ps3"])
                p.A(lambda e, d=d: e.copy(out=Csb[d][:], in_=pTr[:, 0:128]), w=["ps3", nm("Csb")])
                p.A(lambda e, d=d: e.copy(out=qkT[d][:], in_=pTr[:, 128:256]), w=["ps3", nm("qkT")])
                p.V(lambda e, d=d: e.tensor_tensor(out=Pa[d][:], in0=Csb[d][:], in1=ident[:], op=ALU.add),
                    r=[nm("Csb"), "ident"], w=[nm("Pa")])
                p.G(lambda e, d=d: e.tensor_tensor(out=PTa[d][:], in0=CT[d][:], in1=ident[:], op=ALU.add),
                    r=[nm("CT"), "ident"], w=[nm("PTa")])
                Xc, XTc, Xcn, XTcn = Csb, CT, "Csb", "CT"
                Pc, PTc, Pcn, PTcn = Pa, PTa, "Pa", "PTa"
                for i in range(1, 7):
                    Xn, XTn, Xnn, XTnn = (Xa, XTa, "Xa", "XTa") if i % 2 == 1 else (Xb_, XTb, "Xb", "XTb")
                    Pn, PTn, Pnn, PTnn = (Pb, PTb, "Pb", "PTb") if i % 2 == 1 else (Pa, PTa, "Pa", "PTa")
                    last = (i == 6)

                    def mX(e, d=d, Xc=Xc, XTc=XTc, last=last):
                        ins = e.matmul(pX[:, 0:128], lhsT=XTc[d][:], rhs=Xc[d][:], start=True, stop=True)
                        if not last:
                            ins = e.matmul(pX[:, 128:256], lhsT=Xc[d][:], rhs=XTc[d][:], start=True, stop=True)
                        return ins
                    p.T(mX, r=[nm(Xcn), nm(XTcn)], w=["ps4"])
                    p.V(lambda e, d=d, Xn=Xn: e.tensor_copy(out=Xn[d][:], in_=pX[:, 0:128]), w=["ps4", nm(Xnn)])
                    if not last:
                        p.A(lambda e, d=d, XTn=XTn: e.copy(out=XTn[d][:], in_=pX[:, 128:256]), w=["ps4", nm(XTnn)])

                    def mP(e, d=d, Xn=Xn, PTc=PTc, last=last):
                        e.matmul(pP[:, 0:128], lhsT=PTc[d][:], rhs=ident[:], start=True, stop=False)
                        ins = e.matmul(pP[:, 0:128], lhsT=PTc[d][:], rhs=Xn[d][:], start=False, stop=True)
                        if not last:
                            e.matmul(pP[:, 128:256], lhsT=ident[:], rhs=PTc[d][:], start=True, stop=False)
                            ins = e.matmul(pP[:, 128:256], lhsT=Xn[d][:], rhs=PTc[d][:], start=False, stop=True)
                        return ins
                    p.T(mP, r=[nm(PTcn), nm(Xnn), "ident"], w=["ps5"])
                    p.V(lambda e, d=d, Pn=Pn: e.tensor_copy(out=Pn[d][:], in_=pP[:, 0:128]), w=["ps5", nm(Pnn)])
                    if not last:
                        p.A(lambda e, d=d, PTn=PTn: e.copy(out=PTn[d][:], in_=pP[:, 128:256]), w=["ps5", nm(PTnn)])
                    Xc, XTc, Xcn, XTcn = Xn, XTn, Xnn, XTnn
                    Pc, PTc, Pcn, PTcn = Pn, PTn, Pnn, PTnn
                Pfin, Pfn = Pc, Pcn

                def mT2(e, j=j, ts=ts):
                    e.transpose(pT2[:, 0:128], kn[:, j, ts], identb[:])
                    return e.transpose(pT2[:, 128:256], vc[:, j, ts], identb[:])
                p.T(mT2, r=[FK(j), FV(j), "identb"], w=["pT2"])
                p.V(lambda e, d=d, n=n, c=c: e.tensor_tensor(out=E3[d][:, 3:4], in0=beta[:, n, c:c + 1], in1=E3[d][:, 0:1],
                                                             op=ALU.mult), r=["beta"], w=[nm("E3")])
                p.V(lambda e, d=d: e.tensor_scalar(out=rhs_w[d][:], in0=pT2[:, 0:128], scalar1=E3[d][:, 3:4], scalar2=None,
                                                   op0=ALU.mult), r=[nm("E3")], w=["pT2", nm("rhsw")])
                p.V(lambda e, d=d: e.tensor_scalar(out=kdec[d][:], in0=pT2[:, 0:128], scalar1=E3[d][:, 1:2], scalar2=None,
                                                   op0=ALU.mult), r=[nm("E3")], w=["pT2", nm("kdec")])
                p.V(lambda e, d=d, n=n, c=c: e.tensor_scalar(out=rhs_u[d][:], in0=pT2[:, 128:256],
                                                             scalar1=beta[:, n, c:c + 1], scalar2=None, op0=ALU.mult),
                    r=["beta"], w=["pT2", nm("rhsu")])

                def mU(e, d=d, Pfin=Pfin):
                    e.matmul(pD[:, 0:128], lhsT=Pfin[d][:], rhs=rhs_u[d][:], start=True, stop=True)
                    return e.matmul(pD[:, 128:256], lhsT=rhs_w[d][:], rhs=Pfin[d][:], start=True, stop=True)
                p.T(mU, r=[nm(Pfn), nm("rhsu"), nm("rhsw")], w=["ps0"])
                p.A(lambda e, d=d: e.copy(out=u_sb[d][:], in_=pD[:, 0:128]), w=["ps0", nm("usb")])
                p.A(lambda e, d=d: e.copy(out=wT[d][:], in_=pD[:, 128:256]), w=["ps0", nm("wT")])
                Sn, Sbn = f"S{d}{j}", f"Sbf{d}{j}"
                p.T(lambda e, d=d, j=j: e.matmul(pTr[:, 0:128], lhsT=wT[d][:], rhs=Sbf[d][j][:], start=True, stop=True),
                    r=[nm("wT"), Sbn], w=["ps3"])
                p.V(lambda e, d=d: e.tensor_tensor(out=vnew[d][:], in0=u_sb[d][:], in1=pTr[:, 0:128], op=ALU.subtract),
                    r=[nm("usb")], w=["ps3", nm("vnew")])

                def mO(e, d=d, j=j, ts=ts):
                    e.matmul(pO[:, 0:128], lhsT=qn[:, j, ts], rhs=Sbf[d][j][:], start=True, stop=True)
                    e.matmul(pO[:, 128:256], lhsT=qkT[d][:], rhs=vnew[d][:], start=True, stop=True)
                    return e.matmul(pO[:, 256:384], lhsT=kdec[d][:], rhs=vnew[d][:], start=True, stop=True)
                p.T(mO, r=[FQ(j), Sbn, nm("qkT"), nm("vnew"), nm("kdec")], w=["pO"])
                oa = o_acc[:, n, j * 128:(j + 1) * 128]
                p.V(lambda e, d=d, oa=oa: e.scalar_tensor_tensor(out=oa, in0=pO[:, 0:128], scalar=E3[d][:, 0:1], in1=oa,
                                                                 op0=ALU.mult, op1=ALU.add),
                    r=[nm("E3")], w=["pO", "o_acc"])
                p.V(lambda e, oa=oa: e.tensor_tensor(out=oa, in0=pO[:, 128:256], in1=oa, op=ALU.add), w=["pO", "o_acc"])
                p.V(lambda e, d=d, j=j: e.scalar_tensor_tensor(out=S[d][j][:], in0=S[d][j][:], scalar=E3[d][:, 2:3],
                                                               in1=pO[:, 256:384], op0=ALU.mult, op1=ALU.add),
                    r=[nm("E3")], w=["pO", Sn])
                p.A(lambda e, d=d, j=j: e.copy(out=Sbf[d][j][:], in_=S[d][j][:]), r=[Sn], w=[Sbn])
    for n in range(NTA):
        p.dma(zt[:], outT.ap()[n * 128:(n + 1) * 128, 512:768], w=["zt"])
        for j in range(2):
            p.A(lambda e, n=n, j=j: e.activation(out=junk[:], in_=o_acc[:, n, j * 128:(j + 1) * 128], func=AF.Square,
                                                 accum_out=st[:, j:j + 1]), r=["o_acc"], w=["junk", "st"])
        p.V(lambda e: e.tensor_scalar(out=st[:, 2:4], in0=st[:, 0:2], scalar1=1.0 / 128, scalar2=1e-6, op0=ALU.mult,
                                      op1=ALU.add), r=["st"], w=["st"])
        p.A(lambda e: e.activation(out=st[:, 2:4], in_=st[:, 2:4], func=AF.Sqrt), r=["st"], w=["st"])
        p.V(lambda e: e.reciprocal(out=st[:, 4:6], in_=st[:, 2:4]), r=["st"], w=["st"])
        p.A(lambda e: e.activation(out=sg[:], in_=zt[:], func=AF.Silu), r=["zt"], w=["sg"])
        for j in range(2):
            p.V(lambda e, n=n, j=j: e.scalar_tensor_tensor(
                out=tt[:, j * 128:(j + 1) * 128], in0=o_acc[:, n, j * 128:(j + 1) * 128], scalar=st[:, 4 + j:5 + j],
                in1=gn[:], op0=ALU.mult, op1=ALU.mult), r=["o_acc", "st", "gn"], w=["tt"])
        p.V(lambda e: e.tensor_tensor(out=tt[:], in0=tt[:], in1=sg[:], op=ALU.mult), r=["sg"], w=["tt"])

        def mTo(e):
            e.transpose(pX[:, 0:128], tt[:, 0:128], ident[:])
            return e.transpose(pX[:, 128:256], tt[:, 128:256], ident[:])
        p.T(mTo, r=["tt", "ident"], w=["ps4"])
        for j in range(2):
            p.A(lambda e, n=n, j=j: e.copy(out=osb[:, j, n * 128:(n + 1) * 128], in_=pX[:, j * 128:(j + 1) * 128]),
                w=["ps4", "osb"])
    for j in range(2):
        p.dma(gdnT.ap()[j], osb[:, j, :], r=["osb"])
    p.end_stage()


def core_gdn_params(gdn_conv_l, a_log_l, dt_bias_l, hh):
    cw = np.zeros((128, 6, 5), np.float32)
    for ty in range(3):
        for j in range(2):
            ch0 = ty * 512 + (2 * hh + j) * 128
            cw[:, ty * 2 + j, :] = gdn_conv_l[:, ch0:ch0 + 128].T
    ab = np.zeros((1, 8), np.float32)
    for d in range(2):
        for j in range(2):
            ab[0, d * 2 + j] = a_log_l[d, 2 * hh + j]
            ab[0, 4 + d * 2 + j] = dt_bias_l[d, 2 * hh + j]
    return cw.reshape(128, 30), ab


def _single(emit, *a):
    p = Prog()
    emit(p, *a)
    return p.finish()


def build_A1():
    return _single(emit_A1)


def build_A2():
    return _single(emit_A2)


def build_A3():
    return _single(emit_A3)


def build_A4(n_steps=NTA):
    return _single(emit_A4, n_steps)


def build_A5():
    return _single(emit_A5)


def build_stageA():
    p = Prog()
    p.dram("outF", [FROWS, TA], kind="Internal")
    p.dram("outT", [TA, TCOLS], kind="Internal")
    p.dram("naT", [2, 64, TA], kind="Internal")
    p.dram("glaT", [128, TA], kind="Internal")
    p.dram("gdnT", [2, 128, TA], kind="Internal")
    emit_A1(p)
    emit_A2(p)
    emit_A3(p)
    emit_A4(p)
    emit_A5(p)
    return p.finish()


def core_w_out(w_out_l, hh):
    return np.ascontiguousarray(np.concatenate([w_out_l[hh * 128:(hh + 1) * 128], w_out_l[256 + hh * 128:256 + (hh + 1) * 128],
                                                w_out_l[512 + hh * 256:512 + (hh + 1) * 256]], 0))


_CONST = {}


def stageA_inputs(inp, l, b, hh, x_b, ctx_b, vecs):
    if "c" not in _CONST:
        jblk, mask2 = na_consts()
        tri, msk, cos, sin, ptr = gla_consts()
        gtri, negm, strict = gdn_consts()
        _CONST["c"] = dict(ident=np.eye(128, dtype=np.float32), jblk=jblk, mask2=mask2, tri=tri, msk=msk, cos=cos, sin=sin,
                           ptr=ptr, gtri=gtri, negm=negm, strict=strict)
    key = ("w", l, hh)
    if key not in _CONST:
        wF, wT = core_w_in(inp["w_in"][l], hh)
        rpb = np.zeros((2, 15, 127), np.float32)
        rpb[:, :, 48:79] = inp["na_rpb"][l][2 * hh:2 * hh + 2]
        cw, ab = core_gdn_params(inp["gdn_conv"][l], inp["gdn_a_log"][l], inp["gdn_dt_bias"][l], hh)
        _CONST[key] = dict(wF=wF, wT=wT, rpb=rpb, wz=core_wz(inp["gla_w_a2"][l], inp["gla_b_a"][l], hh),
                           gn=np.ascontiguousarray(inp["gla_norm"][l][None]), cw=cw, ab=ab,
                           ggn=np.ascontiguousarray(inp["gdn_norm"][l][None]), wo=core_w_out(inp["w_out"][l], hh))
    m = dict(_CONST["c"])
    m.update(_CONST[key])
    m["xc"] = np.ascontiguousarray(np.concatenate([x_b, ctx_b], 0))
    m["vecs"] = vecs
    return m


_PROGS = {}


def _prog(name, builder):
    if name not in _PROGS:
        _PROGS[name] = builder()
    return _PROGS[name]


def _run(nc, ims):
    res = run_bass_kernel_spmd(nc, ims, core_ids=list(range(8)))
    return res.results


def kernel(x, c, ctx, c_ctx, w_ada, b_ada, w_in, gla_w_a2, gla_b_a, gla_norm, na_rpb, gdn_conv, gdn_a_log,
           gdn_dt_bias, gdn_norm, w_out, ln1_g, ln1_b, w_router, b_router, w_gate_up, b_gate_up, w_down, b_down,
           ln2_g, ln2_b):
    inp = dict(w_in=w_in, gla_w_a2=gla_w_a2, gla_b_a=gla_b_a, gla_norm=gla_norm, na_rpb=na_rpb, gdn_conv=gdn_conv,
               gdn_a_log=gdn_a_log, gdn_dt_bias=gdn_dt_bias, gdn_norm=gdn_norm, w_out=w_out)
    inp = {k: np.asarray(v, dtype=np.float32) for k, v in inp.items()}
    f32 = lambda a: np.ascontiguousarray(np.asarray(a, dtype=np.float32))
    x = f32(x).copy()
    ctx = f32(ctx).copy()
    ident = np.eye(128, dtype=np.float32)
    cc = f32(np.concatenate([np.asarray(c), np.asarray(c_ctx)[None]], 0))
    w_ada = np.asarray(w_ada)
    b_ada = np.asarray(b_ada)
    ims = [{"cc": cc, "wa": f32(w_ada[:, :, k * 768:(k + 1) * 768]), "ba": f32(b_ada[:, k * 768:(k + 1) * 768]),
            "ident": ident} for k in range(8)]
    r0 = _run(_prog("s0", build_stage0), ims)
    m = np.concatenate([r["m"] for r in r0], axis=2)
    ncA = _prog("A", build_stageA)
    ncB = _prog("B", build_stageB)
    for l in range(DEPTH):
        tail = [f32(ln1_g[l]), f32(ln1_b[l]), f32(ln2_g[l]), f32(ln2_b[l])]
        vecs = [np.ascontiguousarray(np.concatenate([m[l, b], m[l, 4]] + tail).reshape(128, 128)) for b in range(NB)]
        imsA = [stageA_inputs(inp, l, b, hh, x[b], ctx[b], vecs[b]) for b in range(NB) for hh in range(2)]
        rA = _run(ncA, imsA)
        wr = f32(w_router[l])
        br = f32(np.asarray(b_router[l])[None])
        wgu = f32(w_gate_up[l])
        bgu = f32(np.asarray(b_gate_up[l]).reshape(512, 128))
        wdn = f32(w_down[l])
        bdn = f32(b_down[l])
        imsB = []
        for b in range(NB):
            for half in range(2):
                rl = slice(half * 2048, (half + 1) * 2048)
                rc = slice(4096 + half * 128, 4096 + (half + 1) * 128)
                cat = lambda a: np.ascontiguousarray(np.concatenate([a[rl], a[rc]], 0))
                xin = np.ascontiguousarray(np.concatenate([x[b][rl], ctx[b][half * 128:(half + 1) * 128]], 0))
                imsB.append({"xin": xin, "pa": cat(rA[2 * b]["part"]), "pb": cat(rA[2 * b + 1]["part"]), "vecs": vecs[b],
                             "ident": ident, "wr": wr, "br": br, "wgu": wgu, "bgu": bgu, "wdn": wdn, "bdn": bdn})
        rB = _run(ncB, imsB)
        for b in range(NB):
            for half in range(2):
                o = rB[2 * b + half]["xout"]
                x[b, half * 2048:(half + 1) * 2048] = o[:2048]
                ctx[b, half * 128:(half + 1) * 128] = o[2048:]
    return x
```
